# Optimizing a Trainium2 kernel written in Bass

```python
import math
import jax, jax.numpy as jnp
from jax import lax
import numpy as np

D_MODEL = 1024
BATCH = 32
SEQ = 2048
DEPTH = 2

PLE_DIM = 256
N_EVEN = (DEPTH + 1) // 2
N_ODD = DEPTH // 2
D_CONV_A = D_MODEL // 2
CONV_A_WIDTH = 3
GDN_HEADS = 4
GDN_DK = D_MODEL // 8
GDN_DV = D_MODEL // 8
GDN_CONV_WIDTH = 4
GDN_CHUNK = 64
DIFF_HEADS = 4
DIFF_DH = D_MODEL // 16
DIFF_QBLK = 128
NUM_BUCKETS = 32
MAX_DISTANCE = 128
D_CONF = D_MODEL // 2
CONF_WIDTH = 31
D_FF = 11 * D_MODEL // 4
N_EXPERTS = 8
TOP_K = 2
D_FF_EXPERT = 7 * D_MODEL // 2
MOE_BLOCK = 256
EPS = 1e-6

EV_WIDTHS = (D_CONV_A, D_CONV_A, D_CONV_A,
             GDN_HEADS * GDN_DK, GDN_HEADS * GDN_DK, GDN_HEADS * GDN_DV,
             GDN_HEADS * GDN_DV, GDN_HEADS, GDN_HEADS)
EV_IN = 3 * D_CONV_A + 2 * GDN_HEADS * GDN_DK + 2 * GDN_HEADS * GDN_DV + 2 * GDN_HEADS
EV_OUT = D_CONV_A + GDN_HEADS * GDN_DV
OD_WIDTHS = (DIFF_HEADS * 2 * DIFF_DH, DIFF_HEADS * 2 * DIFF_DH, DIFF_HEADS * 2 * DIFF_DH, 2 * D_CONF)
OD_IN = 3 * DIFF_HEADS * 2 * DIFF_DH + 2 * D_CONF
OD_OUT = DIFF_HEADS * 2 * DIFF_DH + D_CONF

kernel_name = 'hybrid_conv_gdn_diffattn_conformer_moe'


def rmsnorm(x, g):
    xf = x.astype(jnp.float32)
    y = xf * lax.rsqrt(jnp.mean(xf * xf, axis=-1, keepdims=True) + EPS) * g.astype(jnp.float32)
    return y.astype(x.dtype)


def layernorm(x, g, b):
    xf = x.astype(jnp.float32)
    mu = jnp.mean(xf, axis=-1, keepdims=True)
    var = jnp.mean(jnp.square(xf - mu), axis=-1, keepdims=True)
    y = (xf - mu) * lax.rsqrt(var + EPS) * g.astype(jnp.float32) + b.astype(jnp.float32)
    return y.astype(x.dtype)


def l2norm(x):
    xf = x.astype(jnp.float32)
    return xf * lax.rsqrt(jnp.sum(xf * xf, axis=-1, keepdims=True) + EPS)


def split_cols(u, widths):
    offs = [int(o) for o in np.cumsum(widths)[:-1]]
    return jnp.split(u, offs, axis=-1)


def causal_dwconv(x, w):
    K, C = w.shape
    return lax.conv_general_dilated(x, w[:, None, :].astype(x.dtype), window_strides=(1,),
                                    padding=[(K - 1, 0)], dimension_numbers=('NWC', 'WIO', 'NWC'),
                                    feature_group_count=C)


def gated_delta_chunked(q, k, v, log_a, beta):
    Bb, Ss, H, dk = q.shape
    dv = v.shape[-1]
    C = GDN_CHUNK
    N = Ss // C
    f32 = jnp.float32
    def chunks(t):
        return t.astype(f32).reshape(Bb, N, C, H, -1).transpose(0, 3, 1, 2, 4)
    qc = chunks(q) * (dk ** -0.5)
    kc = chunks(k)
    vc = chunks(v)
    g = log_a.astype(f32).reshape(Bb, N, C, H).transpose(0, 3, 1, 2)
    bt = beta.astype(f32).reshape(Bb, N, C, H).transpose(0, 3, 1, 2)
    gc = jnp.cumsum(g, axis=-1)
    tril = jnp.tril(jnp.ones((C, C), bool))
    strict = jnp.tril(jnp.ones((C, C), bool), -1)
    diff = gc[..., :, None] - gc[..., None, :]
    decay = jnp.where(tril, jnp.exp(jnp.where(tril, diff, 0.0)), 0.0)
    kb = kc * bt[..., None]
    vb = vc * bt[..., None]
    A = jnp.where(strict, jnp.einsum('bhncd,bhnjd->bhncj', kb, kc) * decay, 0.0)
    M = A + jnp.eye(C, dtype=f32)
    u = lax.linalg.triangular_solve(M, vb, left_side=True, lower=True, unit_diagonal=True)
    w = lax.linalg.triangular_solve(M, kb * jnp.exp(gc)[..., None], left_side=True, lower=True,
                                    unit_diagonal=True)
    qk = jnp.einsum('bhncd,bhnjd->bhncj', qc, kc) * decay

    def step(S, xs):
        q_c, k_c, u_c, w_c, g_c, qk_c = xs
        v_new = u_c - jnp.einsum('bhcd,bhde->bhce', w_c, S)
        o = (jnp.einsum('bhcd,bhde->bhce', q_c * jnp.exp(g_c)[..., None], S)
             + jnp.einsum('bhcj,bhje->bhce', qk_c, v_new))
        g_last = g_c[..., -1]
        S = (S * jnp.exp(g_last)[..., None, None]
             + jnp.einsum('bhcd,bhce->bhde', k_c * jnp.exp(g_last[..., None] - g_c)[..., None], v_new))
        return S, o

    xs = (jnp.moveaxis(qc, 2, 0), jnp.moveaxis(kc, 2, 0), jnp.moveaxis(u, 2, 0),
          jnp.moveaxis(w, 2, 0), jnp.moveaxis(gc, 2, 0), jnp.moveaxis(qk, 2, 0))
    S0 = jnp.zeros((Bb, H, dk, dv), f32)
    _, o = lax.scan(step, S0, xs)
    return o.transpose(1, 0, 3, 2, 4).reshape(Bb, Ss, H, dv).astype(v.dtype)


def rel_bucket(rel):
    n = jnp.maximum(rel, 0)
    max_exact = NUM_BUCKETS // 2
    nf = jnp.maximum(n, 1).astype(jnp.float32)
    large = max_exact + (jnp.log(nf / max_exact) / math.log(MAX_DISTANCE / max_exact)
                         * (NUM_BUCKETS - max_exact)).astype(jnp.int32)
    large = jnp.minimum(large, NUM_BUCKETS - 1)
    return jnp.where(n < max_exact, n, large)


def diff_attention(q, k, v, lam, rel_table):
    Bb, Ss, H, _, dh = q.shape
    nq = Ss // DIFF_QBLK
    scale = dh ** -0.5
    qb = q.reshape(Bb, nq, DIFF_QBLK, H, 2, dh).transpose(1, 0, 2, 3, 4, 5)
    k_pos = jnp.arange(Ss, dtype=jnp.int32)

    def block(args):
        qi, iblk = args
        q_pos = iblk * DIFF_QBLK + jnp.arange(DIFF_QBLK, dtype=jnp.int32)
        rel = q_pos[:, None] - k_pos[None, :]
        bias = rel_table.astype(jnp.float32)[rel_bucket(rel)].transpose(2, 0, 1)
        s = jnp.einsum('bqhmd,bkhmd->bmhqk', qi, k).astype(jnp.float32) * scale + bias
        s = jnp.where(rel >= 0, s, -jnp.inf)
        pr = jax.nn.softmax(s, axis=-1)
        a = pr[:, 0] - lam * pr[:, 1]
        return jnp.einsum('bhqk,bkhe->bqhe', a.astype(v.dtype), v)

    o = lax.map(block, (qb, jnp.arange(nq, dtype=jnp.int32)))
    return o.transpose(1, 0, 2, 3, 4).reshape(Bb, Ss, H, 2 * dh)


def even_mixer(hn, w_in, conv_a, gdn_conv, A_log, dt_bias, gdn_norm_g, w_out):
    Bb, Ss, _ = hn.shape
    u = hn @ w_in
    bg, cg, xin, qf, kf, vf, og, af, bf = split_cols(u, EV_WIDTHS)
    ya = bg * causal_dwconv(cg * xin, conv_a)
    qkv = jax.nn.silu(causal_dwconv(jnp.concatenate([qf, kf, vf], axis=-1), gdn_conv))
    qf, kf, vf = split_cols(qkv, (GDN_HEADS * GDN_DK, GDN_HEADS * GDN_DK, GDN_HEADS * GDN_DV))
    q = l2norm(qf.reshape(Bb, Ss, GDN_HEADS, GDN_DK))
    k = l2norm(kf.reshape(Bb, Ss, GDN_HEADS, GDN_DK))
    v = vf.reshape(Bb, Ss, GDN_HEADS, GDN_DV)
    log_a = -jnp.exp(A_log.astype(jnp.float32)) * jax.nn.softplus(af.astype(jnp.float32) + dt_bias.astype(jnp.float32))
    beta = jax.nn.sigmoid(bf.astype(jnp.float32))
    o = gated_delta_chunked(q, k, v, log_a, beta)
    o = rmsnorm(o, gdn_norm_g) * jax.nn.silu(og.reshape(Bb, Ss, GDN_HEADS, GDN_DV))
    y = jnp.concatenate([ya, o.reshape(Bb, Ss, GDN_HEADS * GDN_DV)], axis=-1)
    return y @ w_out


def odd_mixer(hn, w_in, lam_params, diff_norm_g, conf_dw_w, conf_dw_b, conf_ln_g, conf_ln_b,
              w_out, rel_table, lambda_init):
    Bb, Ss, _ = hn.shape
    u = hn @ w_in
    qf, kf, vf, cf = split_cols(u, OD_WIDTHS)
    lp = lam_params.astype(jnp.float32)
    lam = jnp.exp(jnp.sum(lp[0] * lp[1])) - jnp.exp(jnp.sum(lp[2] * lp[3])) + lambda_init
    q = qf.reshape(Bb, Ss, DIFF_HEADS, 2, DIFF_DH)
    k = kf.reshape(Bb, Ss, DIFF_HEADS, 2, DIFF_DH)
    v = vf.reshape(Bb, Ss, DIFF_HEADS, 2 * DIFF_DH)
    o = diff_attention(q, k, v, lam, rel_table)
    o = rmsnorm(o, diff_norm_g) * (1.0 - lambda_init)
    ga, gb = jnp.split(cf, 2, axis=-1)
    c = ga * jax.nn.sigmoid(gb)
    c = causal_dwconv(c, conf_dw_w) + conf_dw_b.astype(c.dtype)
    c = jax.nn.silu(layernorm(c, conf_ln_g, conf_ln_b))
    y = jnp.concatenate([o.reshape(Bb, Ss, DIFF_HEADS * 2 * DIFF_DH), c], axis=-1)
    return y @ w_out


def swiglu(hn, w_gate_up, w_down):
    g, u = jnp.split(hn @ w_gate_up, 2, axis=-1)
    return (jax.nn.silu(g) * u) @ w_down


def moe_swiglu(hn, w_router, w_gate_up, w_down):
    Bb, Ss, D = hn.shape
    T = Bb * Ss
    xt = hn.reshape(T, D)
    logits = (xt @ w_router).astype(jnp.float32)
    top_val, top_idx = lax.top_k(logits, TOP_K)
    gates = jax.nn.softmax(top_val, axis=-1)
    e_flat = top_idx.reshape(-1).astype(jnp.int32)
    tok_flat = jnp.repeat(jnp.arange(T, dtype=jnp.int32), TOP_K)
    g_flat = gates.reshape(-1)
    order = jnp.argsort(e_flat)
    e_s, tok_s, g_s = e_flat[order], tok_flat[order], g_flat[order]
    counts = jnp.zeros((N_EXPERTS,), jnp.int32).at[e_flat].add(1)
    padded = (counts + MOE_BLOCK - 1) // MOE_BLOCK * MOE_BLOCK
    start = jnp.cumsum(counts) - counts
    pend = jnp.cumsum(padded)
    pstart = pend - padded
    dest = pstart[e_s] + (jnp.arange(T * TOP_K, dtype=jnp.int32) - start[e_s])
    P = T * TOP_K + N_EXPERTS * MOE_BLOCK
    NB = P // MOE_BLOCK
    row_tok = jnp.full((P,), T, jnp.int32).at[dest].set(tok_s)
    xpad = jnp.concatenate([xt, jnp.zeros((1, D), xt.dtype)], axis=0)
    rows = xpad[row_tok].reshape(NB, MOE_BLOCK, D)
    blk_e = jnp.minimum(jnp.searchsorted(pend, jnp.arange(NB, dtype=jnp.int32) * MOE_BLOCK, side='right'),
                        N_EXPERTS - 1)

    def expert_block(args):
        xb, e = args
        g, u = jnp.split(xb @ w_gate_up[e], 2, axis=-1)
        return (jax.nn.silu(g) * u) @ w_down[e]

    y = lax.map(expert_block, (rows, blk_e)).reshape(P, D)
    contrib = y[dest] * g_s[:, None].astype(y.dtype)
    out = jnp.zeros((T, D), y.dtype).at[tok_s].add(contrib)
    return out.reshape(Bb, Ss, D)


def per_layer_embed(h, p_i, g_norm, w_proj, w_gate):
    gate = jax.nn.sigmoid((rmsnorm(h, g_norm) @ w_gate).astype(jnp.float32)).astype(h.dtype)
    return (p_i.astype(h.dtype) @ w_proj) * gate


def setup_inputs(seed: int = 0) -> dict:
    key = jax.random.key(seed)
    ks = jax.random.split(key, 40)
    f32 = jnp.float32
    def nrm(k, shape, scale):
        return jax.random.normal(k, shape, f32) * scale
    def gain(k, shape):
        return 1.0 + 0.02 * jax.random.normal(k, shape, f32)
    D = D_MODEL
    A = jax.random.uniform(ks[9], (N_EVEN, GDN_HEADS), f32, 1.0, 16.0)
    dt = jnp.exp(jax.random.uniform(ks[10], (N_EVEN, GDN_HEADS), f32, math.log(1e-3), math.log(1e-1)))
    return {
        'x': nrm(ks[0], (BATCH, SEQ, D), 1.0),
        'p': nrm(ks[1], (DEPTH, BATCH, SEQ, PLE_DIM), 1.0),
        'norm_mix_g': gain(ks[2], (DEPTH, D)),
        'norm_ffn_g': gain(ks[3], (DEPTH, D)),
        'norm_ple_g': gain(ks[4], (DEPTH, D)),
        'final_norm_g': gain(ks[5], (D,)),
        'ev_w_in': nrm(ks[6], (N_EVEN, D, EV_IN), D ** -0.5),
        'ev_conv_a': nrm(ks[7], (N_EVEN, CONV_A_WIDTH, D_CONV_A), CONV_A_WIDTH ** -0.5),
        'ev_gdn_conv': nrm(ks[8], (N_EVEN, GDN_CONV_WIDTH, 2 * GDN_HEADS * GDN_DK + GDN_HEADS * GDN_DV), GDN_CONV_WIDTH ** -0.5),
        'ev_gdn_A_log': jnp.log(A),
        'ev_gdn_dt_bias': dt + jnp.log(-jnp.expm1(-dt)),
        'ev_gdn_norm_g': gain(ks[11], (N_EVEN, GDN_DV)),
        'ev_w_out': nrm(ks[12], (N_EVEN, EV_OUT, D), EV_OUT ** -0.5),
        'od_w_in': nrm(ks[13], (N_ODD, D, OD_IN), D ** -0.5),
        'od_lambda': nrm(ks[14], (N_ODD, 4, DIFF_DH), 0.1),
        'od_diff_norm_g': gain(ks[15], (N_ODD, 2 * DIFF_DH)),
        'od_conf_dw_w': nrm(ks[16], (N_ODD, CONF_WIDTH, D_CONF), CONF_WIDTH ** -0.5),
        'od_conf_dw_b': nrm(ks[17], (N_ODD, D_CONF), 0.02),
        'od_conf_ln_g': gain(ks[18], (N_ODD, D_CONF)),
        'od_conf_ln_b': nrm(ks[19], (N_ODD, D_CONF), 0.02),
        'od_w_out': nrm(ks[20], (N_ODD, OD_OUT, D), OD_OUT ** -0.5),
        'rel_bias': nrm(ks[21], (NUM_BUCKETS, DIFF_HEADS), 0.1),
        'ffn_w_gate_up': nrm(ks[22], (N_EVEN, D, 2 * D_FF), D ** -0.5),
        'ffn_w_down': nrm(ks[23], (N_EVEN, D_FF, D), D_FF ** -0.5),
        'moe_router': nrm(ks[24], (N_ODD, D, N_EXPERTS), D ** -0.5),
        'moe_w_gate_up': nrm(ks[25], (N_ODD, N_EXPERTS, D, 2 * D_FF_EXPERT), D ** -0.5),
        'moe_w_down': nrm(ks[26], (N_ODD, N_EXPERTS, D_FF_EXPERT, D), D_FF_EXPERT ** -0.5),
        'ple_w_proj': nrm(ks[27], (DEPTH, PLE_DIM, D), PLE_DIM ** -0.5),
        'ple_w_gate': nrm(ks[28], (DEPTH, D, D), D ** -0.5),
    }


def reference(x, p, norm_mix_g, norm_ffn_g, norm_ple_g, final_norm_g,
              ev_w_in, ev_conv_a, ev_gdn_conv, ev_gdn_A_log, ev_gdn_dt_bias, ev_gdn_norm_g, ev_w_out,
              od_w_in, od_lambda, od_diff_norm_g, od_conf_dw_w, od_conf_dw_b, od_conf_ln_g, od_conf_ln_b,
              od_w_out, rel_bias, ffn_w_gate_up, ffn_w_down, moe_router, moe_w_gate_up, moe_w_down,
              ple_w_proj, ple_w_gate):
    h = x
    for i in range(DEPTH):
        j = i // 2
        if i % 2 == 0:
            h = h + even_mixer(rmsnorm(h, norm_mix_g[i]), ev_w_in[j], ev_conv_a[j], ev_gdn_conv[j],
                               ev_gdn_A_log[j], ev_gdn_dt_bias[j], ev_gdn_norm_g[j], ev_w_out[j])
            h = h + swiglu(rmsnorm(h, norm_ffn_g[i]), ffn_w_gate_up[j], ffn_w_down[j])
        else:
            lambda_init = 0.8 - 0.6 * math.exp(-0.3 * i)
            h = h + odd_mixer(rmsnorm(h, norm_mix_g[i]), od_w_in[j], od_lambda[j], od_diff_norm_g[j],
                              od_conf_dw_w[j], od_conf_dw_b[j], od_conf_ln_g[j], od_conf_ln_b[j],
                              od_w_out[j], rel_bias, lambda_init)
            h = h + moe_swiglu(rmsnorm(h, norm_ffn_g[i]), moe_router[j], moe_w_gate_up[j], moe_w_down[j])
        h = h + per_layer_embed(h, p[i], norm_ple_g[i], ple_w_proj[i], ple_w_gate[i])
    return rmsnorm(h, final_norm_g)
```

```python
import math
from contextlib import ExitStack

import numpy as np
import concourse.bass as bass
import concourse.mybir as mybir
from concourse.bass_utils import run_bass_kernel_spmd

F32 = mybir.dt.float32
BF16 = mybir.dt.bfloat16
AF = mybir.ActivationFunctionType
ALU = mybir.AluOpType
AX = mybir.AxisListType

PE, ACT, DVE, POOL, SP = "pe", "act", "dve", "pool", "sp"
ENGS = (PE, ACT, DVE, POOL, SP)

S = 2048
D = 1024
NT = 16
NB = 4
KC = 8
NCORES = 8
SEQ_PER_CORE = 4
EPS = 1e-6
EV_IN = 3592
OD_IN = 2560
D_FF = 2816
D_FFE = 3584
NEXP = 8
LAMBDA_INIT = 0.8 - 0.6 * math.exp(-0.3 * 1)
NEGBIG = -30000.0

SM_G = 0
SM_CONVA = 56
SM_GCONV = 68
SM_GDNG = 116
SM_DIFFG = 117
SM_DWW = 118
SM_DWB = 242
SM_LNG = 246
SM_LNB = 250
SM_RT = 254
SM_ALOG = 318
SM_DTB = 322
SM_LAM = 326
SM_TBL = 582
SM_N = 710
C_ID = 0
C_U = 128
C_NEGU = 256
C_POSL = 384
C_NEGM = 512
C_N = 768


import os as _os
PEPE = bool(int(_os.environ.get("KPEPE", "0")))
GDN_BARRIER = bool(int(_os.environ.get("KGDNBAR", "0")))


class Buf:
    __slots__ = ("name", "w", "r")

    def __init__(self, name=""):
        self.name = name
        self.w = None
        self.r = []


def bufs(n, name=""):
    return [Buf(f"{name}{i}") for i in range(n)]


class Prog:
    NDMA = 32

    def __init__(self, nc):
        self.nc = nc
        self.streams = {e: [] for e in ENGS}
        self.cnt = {e: 0 for e in ENGS}
        self.known = {e: {} for e in ENGS}
        self.dma_cnt = [0] * self.NDMA
        self.dma_rr = 0
        self.dma_rr2 = [0, 0]

    def _deps(self, eng, reads, writes):
        deps = {}

        def add(ev):
            if ev is None:
                return
            k, v = ev
            if k == PE and eng == PE and not PEPE:
                return
            if deps.get(k, 0) < v:
                deps[k] = v
        for b in reads:
            add(b.w)
        for b in writes:
            add(b.w)
            for ev in b.r:
                add(ev)
        kn = self.known[eng]
        out = []
        for k, v in deps.items():
            if kn.get(k, 0) < v:
                kn[k] = v
                out.append((k, v))
        return out

    def _mark(self, ev, reads, writes):
        for b in reads:
            b.r.append(ev)
            if len(b.r) > 64:
                m = {}
                for k, v in b.r:
                    if m.get(k, 0) < v:
                        m[k] = v
                b.r = list(m.items())
        for b in writes:
            b.w = ev
            b.r = []

    def op(self, eng, fn, reads=(), writes=()):
        waits = self._deps(eng, reads, writes)
        self.cnt[eng] += 1
        ev = (eng, self.cnt[eng])
        self.streams[eng].append(("op", fn, waits))
        self._mark(ev, reads, writes)
        return ev

    def dma(self, q, out, in_, reads=(), writes=()):
        waits = self._deps(q, reads, writes)
        g = 0 if q == POOL else 1
        half = self.NDMA // 2
        i = g * half + self.dma_rr2[g]
        self.dma_rr2[g] = (self.dma_rr2[g] + 1) % half
        key = ("dma", i)
        prev = self.dma_cnt[i]
        if prev and self.known[q].get(key, 0) < prev:
            self.known[q][key] = prev
            waits.append((key, prev))
        self.dma_cnt[i] = prev + 16
        ev = (key, prev + 16)
        self.streams[q].append(("dma", (out, in_, i), waits))
        self._mark(ev, reads, writes)
        return ev

    def barrier(self):
        for e in ENGS:
            waits = []
            kn = self.known[e]
            for f in ENGS:
                if f != e and self.cnt[f] > kn.get(f, 0):
                    kn[f] = self.cnt[f]
                    waits.append((f, self.cnt[f]))
            for i in range(self.NDMA):
                key = ("dma", i)
                if self.dma_cnt[i] > kn.get(key, 0):
                    kn[key] = self.dma_cnt[i]
                    waits.append((key, self.dma_cnt[i]))
            if waits:
                self.streams[e].append(("wait", None, waits))

    def emit(self, block, sems, dsems):
        def semof(k):
            return dsems[k[1]] if isinstance(k, tuple) else sems[k]

        def run(name, e):
            for kind, payload, waits in self.streams[name]:
                for k, v in waits:
                    e.wait_ge(semof(k), v)
                if kind == "op":
                    payload(e).then_inc(sems[name], 1)
                elif kind == "dma":
                    out, in_, i = payload
                    e.dma_start(out=out, in_=in_).then_inc(dsems[i], 16)

        @block.sync
        def _(e):
            run(SP, e)

        @block.scalar
        def _(e):
            run(ACT, e)

        @block.vector
        def _(e):
            run(DVE, e)

        @block.gpsimd
        def _(e):
            run(POOL, e)

        @block.tensor
        def _(e):
            run(PE, e)


class RPool:
    def __init__(self, sc, name, n, shape, dt, nb=1):
        self.tiles = [sc.sb(f"{name}{i}", shape, dt) for i in range(n)]
        self.bufs = [bufs(nb, f"{name}{i}_") for i in range(n)]
        self.i = 0

    def next(self):
        t, b = self.tiles[self.i], self.bufs[self.i]
        self.i = (self.i + 1) % len(self.tiles)
        return t, b


class Scope:
    def __init__(self, k):
        self.k = k
        self.es = ExitStack()

    def __enter__(self):
        self.es.__enter__()
        return self

    def __exit__(self, *a):
        self.k.P.barrier()
        return self.es.__exit__(*a)

    def sb(self, name, shape, dt):
        self.k.uid += 1
        return self.es.enter_context(self.k.nc.sbuf_tensor(f"{name}_{self.k.uid}", shape, dt))


class K:
    def __init__(self, nc, nseq, stop_after):
        self.nc = nc
        self.P = Prog(nc)
        self.uid = 0
        self.nseq = nseq
        self.stop_after = stop_after
        import os
        self.dbg = int(os.environ.get("KDBG", "0"))

    def mm(self, out, lhsT, rhs, start, stop, r, w):
        self.P.op(PE, lambda e: e.matmul(out, lhsT=lhsT, rhs=rhs, start=start, stop=stop), r, w)

    def tr(self, out, in_, r, w):
        idn = self.ident
        self.P.op(PE, lambda e: e.transpose(out=out, in_=in_, identity=idn), r + [self.cB], w)

    def act(self, out, in_, func, r, w, bias=None, scale=None):
        kw = {}
        if bias is not None:
            kw["bias"] = bias
        if scale is not None:
            kw["scale"] = scale
        self.P.op(ACT, lambda e: e.activation(out=out, in_=in_, func=func, **kw), r, w)

    def cp(self, eng, out, in_, r, w):
        if eng == ACT:
            self.P.op(ACT, lambda e: e.copy(out=out, in_=in_), r, w)
        else:
            self.P.op(eng, lambda e: e.tensor_copy(out=out, in_=in_), r, w)

    def tt(self, out, in0, in1, op, r, w, eng=DVE):
        self.P.op(eng, lambda e: e.tensor_tensor(out=out, in0=in0, in1=in1, op=op), r, w)

    def ts(self, out, in0, s1, s2, op0, op1, r, w, eng=DVE):
        if s2 is None:
            self.P.op(eng, lambda e: e.tensor_scalar(out=out, in0=in0, scalar1=s1, scalar2=None, op0=op0), r, w)
        else:
            self.P.op(eng, lambda e: e.tensor_scalar(out=out, in0=in0, scalar1=s1, scalar2=s2, op0=op0, op1=op1), r, w)

    def stt(self, out, in0, scalar, in1, op0, op1, r, w, eng=DVE):
        self.P.op(eng, lambda e: e.scalar_tensor_tensor(out=out, in0=in0, scalar=scalar, in1=in1, op0=op0, op1=op1), r, w)

    def recip(self, out, in_, r, w):
        self.P.op(DVE, lambda e: e.reciprocal(out=out, in_=in_), r, w)

    def memset(self, eng, ap, val, w):
        self.P.op(eng, lambda e: e.memset(ap, val), [], w)

    def psn(self):
        i = self.ps_i
        self.ps_i = (i + 1) % len(self.ps_rot)
        j = self.ps_rot[i]
        return self.ps[j], self.psB[j]

    def scope(self):
        return Scope(self)

    def wload(self, tile_ap, dram_ap, wbuf):
        self.P.dma(POOL, tile_ap, dram_ap, [], [wbuf])

    def rstd_from(self, ps_ap, psB, out_ap, outB, inv_n, tmp_ap, tmpB):
        self.act(tmp_ap, ps_ap, AF.Sqrt, [psB], [tmpB], bias=EPS, scale=inv_n)
        self.recip(out_ap, tmp_ap, [tmpB], [outB])

    def rmsnorm(self, gi, dst_bf=True, cb32=None, cb_sq=None):
        with self.scope() as sc:
            sqp = RPool(sc, "sq", 2, [128, KC, 512], BF16)
            rsp = RPool(sc, "rs", 2, [128, 512], F32)
            tmp = RPool(sc, "rt", 2, [128, 512], F32)
            h32p = RPool(sc, "h32", 2, [128, KC, 512], F32) if cb32 is not None else None
            for tb in range(NB):
                tsl = slice(tb * 512, (tb + 1) * 512)
                sq, sqB = sqp.next()
                hB = [self.hB[c][tb] for c in range(KC)]
                self.act(sq[:], self.hT[:, :, tsl], AF.Square, hB, sqB)
                ps, psB = self.psn()
                for c in range(KC):
                    self.mm(ps[:], self.ones_bf[:], sq[:, c, :], c == 0, c == KC - 1, sqB + [self.cB], [psB])
                if cb_sq is not None:
                    cb_sq(tb, sq, sqB)
                rs, rsB = rsp.next()
                t_, tB = tmp.next()
                self.rstd_from(ps[:], psB, rs[:], rsB[0], 1.0 / D, t_[:], tB[0])
                if dst_bf:
                    for c in range(KC):
                        self.stt(self.hnT[:, c, tsl], self.hT[:, c, tsl], self.sm[:, SM_G + gi * 8 + c:SM_G + gi * 8 + c + 1],
                                 rs[:], ALU.mult, ALU.mult, [self.hB[c][tb], rsB[0], self.cB], [self.hnB[tb]])
                if cb32 is not None:
                    h32, h32B = h32p.next()
                    for c in range(KC):
                        self.stt(h32[:, c, :], self.hT[:, c, tsl], self.sm[:, SM_G + gi * 8 + c:SM_G + gi * 8 + c + 1],
                                 rs[:], ALU.mult, ALU.mult, [self.hB[c][tb], rsB[0], self.cB], h32B)
                    cb32(tb, h32, h32B[0])

    def proj(self, ps, psB, wt, wB, col0, tb, src=None, srcB=None, nk=KC):
        src = self.hnT if src is None else src
        srcB = self.hnB[tb] if srcB is None else srcB
        tsl = slice(tb * 512, (tb + 1) * 512)
        for kc in range(nk):
            self.mm(ps[:], wt[:, kc, col0:col0 + 128], src[:, kc, tsl], kc == 0, kc == nk - 1, [wB, srcB], [psB])

    def outproj(self, w_dram, r0):
        with self.scope() as sc:
            wo = sc.sb("wo", [128, 4, D], BF16)
            woB = Buf("wo")
            self.wload(wo[:], w_dram[r0:r0 + 512, :].rearrange("(j p) n -> p j n", p=128), woB)
            for d in range(KC):
                for tb in range(NB):
                    tsl = slice(tb * 512, (tb + 1) * 512)
                    ps, psB = self.psn()
                    for j in range(4):
                        self.mm(ps[:], wo[:, j, d * 128:(d + 1) * 128], self.yT[:, j, tsl], j == 0, j == 3,
                                [woB, self.yB[j][tb]], [psB])
                    self.tt(self.hT[:, d, tsl], self.hT[:, d, tsl], ps[:], ALU.add, [psB, self.hB[d][tb]], [self.hB[d][tb]])

    def swiglu_multi(self, jobs, pools, after_first_load=None):
        wp, wdp, sgp = pools
        groups = []
        for ji, (wgu, wd, F, gs) in enumerate(jobs):
            nf = F // 128
            for f0 in range(0, nf, 4):
                groups.append((ji, f0, min(4, nf - f0)))

        def load(ji, f0, nfc):
            wgu, wd, F, gs = jobs[ji]
            wg, wgB = wp.next()
            wu, wuB = wp.next()
            wdt, wdB = wdp.next()
            c0 = f0 * 128
            self.wload(wg[:, :, 0:nfc * 128], wgu[:, c0:c0 + nfc * 128].rearrange("(kc p) n -> p kc n", p=128), wgB[0])
            self.wload(wu[:, :, 0:nfc * 128], wgu[:, F + c0:F + c0 + nfc * 128].rearrange("(kc p) n -> p kc n", p=128), wuB[0])
            self.wload(wdt[:, 0:nfc, :], wd[c0:c0 + nfc * 128, :].rearrange("(j p) n -> p j n", p=128), wdB[0])
            return wg, wgB, wu, wuB, wdt, wdB

        nxt = load(*groups[0])
        if after_first_load is not None:
            after_first_load()
        gate = None
        for gi, (ji, f0, nfc) in enumerate(groups):
            wg, wgB, wu, wuB, wdt, wdB = nxt
            nxt = load(*groups[gi + 1]) if gi + 1 < len(groups) else None
            if f0 == 0:
                gs = jobs[ji][3]
                gate = gs() if gs is not None else None
            for j in range(nfc):
                for tb in range(NB):
                    tsl = slice(tb * 512, (tb + 1) * 512)
                    pg, pgB = self.psn()
                    self.proj(pg, pgB, wg, wgB[0], j * 128, tb)
                    pu, puB = self.psn()
                    self.proj(pu, puB, wu, wuB[0], j * 128, tb)
                    sg, sgB = sgp.next()
                    self.act(sg[:], pg[:], AF.Silu, [pgB], sgB)
                    if gate is not None:
                        self.tt(sg[:], sg[:], gate[0][:, tsl], ALU.mult, [sgB[0], gate[1]], sgB)
                    self.tt(self.yT[:, j, tsl], sg[:], pu[:], ALU.mult, [sgB[0], puB], [self.yB[j][tb]])
            for d in range(KC):
                for tb in range(NB):
                    tsl = slice(tb * 512, (tb + 1) * 512)
                    ps, psB = self.psn()
                    for j in range(nfc):
                        self.mm(ps[:], wdt[:, j, d * 128:(d + 1) * 128], self.yT[:, j, tsl], j == 0, j == nfc - 1,
                                [wdB[0], self.yB[j][tb]], [psB])
                    self.tt(self.hT[:, d, tsl], self.hT[:, d, tsl], ps[:], ALU.add, [psB, self.hB[d][tb]], [self.hB[d][tb]])

    def ffn_pools(self, sc):
        return (RPool(sc, "wp", 4, [128, KC, 512], BF16), RPool(sc, "wdp", 2, [128, 4, D], BF16),
                RPool(sc, "sg", 4, [128, 512], F32))

    def load_x(self, b):
        with self.scope() as sc:
            xp = RPool(sc, "xin", 3, [128, D], F32)
            for tt in range(NT):
                xt, xB = xp.next()
                self.P.dma(SP, xt[:], self.x[b, tt * 128:(tt + 1) * 128, :], [], xB)
                for half in range(2):
                    ps, psB = self.psn()
                    for j in range(4):
                        c = half * 4 + j
                        self.tr(ps[:, j * 128:(j + 1) * 128], xt[:, c * 128:(c + 1) * 128], xB, [psB])
                    wB = [self.hB[half * 4 + j][tt // 4] for j in range(4)]
                    eng = ACT if half == 0 else DVE
                    self.cp(eng, self.hT[:, half * 4:half * 4 + 4, tt * 128:(tt + 1) * 128],
                            ps[:].rearrange("p (j t) -> p j t", j=4), [psB], wB)

    def store_out(self, b, final):
        with self.scope() as sc:
            op_ = RPool(sc, "ost", 3, [128, D], F32)

            def emit_block(tb, src, srcB):
                for t4 in range(4):
                    tt = tb * 4 + t4
                    ot, oB = op_.next()
                    for half in range(2):
                        ps, psB = self.psn()
                        for j in range(4):
                            c = half * 4 + j
                            self.tr(ps[:, j * 128:(j + 1) * 128], src(c, t4), srcB, [psB])
                        eng = ACT if half == 0 else DVE
                        self.cp(eng, ot[:, half * 512:(half + 1) * 512], ps[:], [psB], oB)
                    self.P.dma(SP, self.out[b, tt * 128:(tt + 1) * 128, :], ot[:], oB, [self.outB])

            if final:
                self.rmsnorm(6, dst_bf=False,
                             cb32=lambda tb, h32, hB_: emit_block(tb, lambda c, t4: h32[:, c, t4 * 128:(t4 + 1) * 128], [hB_]))
            else:
                for tb in range(NB):
                    emit_block(tb, lambda c, t4, tb=tb: self.hT[:, c, tb * 512 + t4 * 128: tb * 512 + (t4 + 1) * 128],
                               [self.hB[c][tb] for c in range(KC)])

    def mixer0(self):
        w_in = self.w["ev_w_in"]
        self.rmsnorm(0)
        sm = self.sm
        if self.dbg == 1:
            return
        with self.scope() as sc:
            wts = []
            for i in range(3):
                t = sc.sb("wa", [128, KC, 512], BF16)
                tB = Buf("wa")
                self.wload(t[:], w_in[:, i * 512:(i + 1) * 512].rearrange("(kc p) n -> p kc n", p=128), tB)
                wts.append((t, tB))
            bgp = RPool(sc, "bg", 2, [128, S], F32)
            zp = RPool(sc, "z", 2, [128, 2 + S], F32)
            xip = RPool(sc, "xi", 2, [128, 512], F32)
            accp = RPool(sc, "acc", 2, [128, S], F32)
            for t, tB in zip(zp.tiles, zp.bufs):
                self.memset(DVE, t[:, 0:2], 0.0, tB)
            for c in range(4):
                bg, bgB = bgp.next()
                z, zB = zp.next()
                for tb in range(NB):
                    tsl = slice(tb * 512, (tb + 1) * 512)
                    p0, p0B = self.psn()
                    self.proj(p0, p0B, wts[0][0], wts[0][1], c * 128, tb)
                    self.cp(ACT, bg[:, tsl], p0[:], [p0B], bgB)
                    p2, p2B = self.psn()
                    self.proj(p2, p2B, wts[2][0], wts[2][1], c * 128, tb)
                    xi, xiB = xip.next()
                    self.cp(ACT, xi[:], p2[:], [p2B], xiB)
                    p1, p1B = self.psn()
                    self.proj(p1, p1B, wts[1][0], wts[1][1], c * 128, tb)
                    self.tt(z[:, 2 + tb * 512:2 + (tb + 1) * 512], p1[:], xi[:], ALU.mult, [p1B, xiB[0]], zB)
                acc, accB = accp.next()
                cw = lambda j: sm[:, SM_CONVA + c * 3 + j:SM_CONVA + c * 3 + j + 1]
                self.ts(acc[:], z[:, 0:S], cw(0), None, ALU.mult, None, [zB[0], self.cB], accB)
                self.stt(acc[:], z[:, 1:1 + S], cw(1), acc[:], ALU.mult, ALU.add, [zB[0], accB[0], self.cB], accB)
                self.stt(acc[:], z[:, 2:2 + S], cw(2), acc[:], ALU.mult, ALU.add, [zB[0], accB[0], self.cB], accB)
                for tb in range(NB):
                    tsl = slice(tb * 512, (tb + 1) * 512)
                    self.tt(self.yT[:, c, tsl], acc[:, tsl], bg[:, tsl], ALU.mult, [accB[0], bgB[0]], [self.yB[c][tb]])
        if self.dbg == 2:
            return
        self.outproj(self.w["ev_w_out"], 0)
        if self.dbg == 3:
            return
        self.gdn()
        if self.dbg in (4, 5, 6, 7, 8, 71, 72, 73, 74, 75, 711, 712, 713, 714, 76, 77, 78, 79):
            return
        self.outproj(self.w["ev_w_out"], 512)

    def gdn(self):
        w_in = self.w["ev_w_in"]
        sm = self.sm
        cst = self.cst
        cB = self.cB
        SC = 128 ** -0.5
        with self.scope() as sc:
            gw = sc.sb("gw", [128, KC, 8], BF16)
            gwB = Buf()
            self.wload(gw[:], w_in[:, 3584:3592].rearrange("(kc p) n -> p kc n", p=128), gwB)
            graw = sc.sb("graw", [128, NT, 8], F32)
            G = {n: (sc.sb(n, [128, NT, 4], F32), Buf(n)) for n in
                 ["g", "beta", "nbeta", "gc", "gtot", "egc", "eglast", "ekd", "nbeg", "t1"]}
            grB = Buf()
            ps, psB = self.psn()
            for tt in range(NT):
                for kc in range(KC):
                    self.mm(ps[:, tt * 8:(tt + 1) * 8], self.hnT[:, kc, tt * 128:(tt + 1) * 128], gw[:, kc, :],
                            kc == 0, kc == KC - 1, [gwB, self.hnB[tt // 4]], [psB])
            self.cp(DVE, graw[:], ps[:, 0:128].rearrange("p (t e) -> p t e", e=8), [psB], [grB])
            g, gB = G["g"]
            t1, t1B = G["t1"]
            bro = lambda c0: sm[:, c0:c0 + 4].unsqueeze(1).to_broadcast([128, NT, 4])
            self.tt(t1[:], graw[:, :, 0:4], bro(SM_DTB), ALU.add, [grB, cB], [t1B])
            self.act(t1[:], t1[:], AF.Exp, [t1B], [t1B])
            self.act(t1[:], t1[:], AF.Ln, [t1B], [t1B], bias=1.0)
            self.tt(g[:], t1[:], self.negA[:].unsqueeze(1).to_broadcast([128, NT, 4]), ALU.mult, [t1B, cB], [gB])
            beta, betaB = G["beta"]
            nbeta, nbetaB = G["nbeta"]
            self.act(beta[:], graw[:, :, 4:8], AF.Sigmoid, [grB], [betaB])
            self.ts(nbeta[:], beta[:], -1.0, None, ALU.mult, None, [betaB], [nbetaB])
            gc, gcB = G["gc"]
            gtot, gtotB = G["gtot"]
            ps, psB = self.psn()
            gflat = g[:].rearrange("p t e -> p (t e)")
            self.mm(ps[:, 0:64], cst[:, C_U:C_U + 128], gflat, True, True, [gB, cB], [psB])
            self.cp(DVE, gc[:].rearrange("p t e -> p (t e)"), ps[:, 0:64], [psB], [gcB])
            ps, psB = self.psn()
            self.mm(ps[:, 0:64], self.ones32[:], gflat, True, True, [gB, cB], [psB])
            self.cp(DVE, gtot[:].rearrange("p t e -> p (t e)"), ps[:, 0:64], [psB], [gtotB])
            egc, egcB = G["egc"]
            eglast, eglB = G["eglast"]
            ekd, ekdB = G["ekd"]
            nbeg, nbegB = G["nbeg"]
            self.act(egc[:], gc[:], AF.Exp, [gcB], [egcB])
            self.act(eglast[:], gtot[:], AF.Exp, [gtotB], [eglB])
            self.tt(ekd[:], gtot[:], gc[:], ALU.subtract, [gtotB, gcB], [ekdB])
            self.act(ekd[:], ekd[:], AF.Exp, [ekdB], [ekdB])
            self.tt(nbeg[:], nbeta[:], egc[:], ALU.mult, [nbetaB, egcB], [nbegB])

            if self.dbg == 4:
                return
            wtp = RPool(sc, "wh", 1, [128, KC, 512], BF16, nb=4)
            Wa = sc.sb("Wa", [128, 4 + S], BF16)
            WaB = Buf()
            Wb = sc.sb("Wb", [128, S], F32)
            WbB = Buf()
            self.memset(DVE, Wa[:, 0:4], 0.0, [WaB])
            dgc = sc.sb("dgc", [128, 4, 128], BF16)
            dgcB = Buf()
            qT = sc.sb("qT", [128, S], BF16)
            kT = sc.sb("kT", [128, S], BF16)
            vT = sc.sb("vT", [128, S], BF16)
            sqb = sc.sb("sqb", [128, S], BF16)
            qTB, kTB, vTB, sqbB = bufs(NB, "qT"), bufs(NB, "kT"), bufs(NB, "vT"), bufs(NB, "sqb")
            ktok = sc.sb("ktok", [128, NT, 128], BF16)
            vtok = sc.sb("vtok", [128, NT, 128], BF16)
            ktokB, vtokB = bufs(2, "ktok"), bufs(2, "vtok")
            oT = Wb[:, 0:S]
            oTB = [WbB] * NT
            rsp = RPool(sc, "grs", 1, [128, 512], F32)
            rtp = RPool(sc, "grt", 1, [128, 512], F32)
            KU = 3
            slots = []
            for k_ in range(KU):
                slots.append({
                    "f32p": RPool(sc, f"u32_{k_}_", 5, [128, 128], F32),
                    "xyp": RPool(sc, f"xy_{k_}_", 3, [128, 256], BF16),
                    "pp": RPool(sc, f"pp_{k_}_", 3, [128, 128], BF16),
                    "up": {n: RPool(sc, f"{n}_{k_}_", 2, [128, 128], BF16) for n in ["TT", "wTn", "vb", "qkT", "qgT", "kdec", "nkbg"]},
                    "upi": {n: 0 for n in ["TT", "wTn", "vb", "qkT", "qgT", "kdec", "nkbg"]},
                    "banks": [2 * k_, 2 * k_ + 1],
                })
            vnp = RPool(sc, "vnew", 2, [128, 128], BF16)
            S32 = sc.sb("S32", [128, 128], F32)
            Sb = sc.sb("Sb", [128, 128], BF16)
            S32B, SbB = Buf(), Buf()

            def l2n_block(tb, dst, dstB):
                tsl = slice(tb * 512, (tb + 1) * 512)
                self.act(sqb[:, tsl], Wb[:, tsl], AF.Square, [WbB], [sqbB[tb]])
                ps, psB = self.psn()
                self.mm(ps[:], self.ones_bf[:], sqb[:, tsl], True, True, [sqbB[tb], cB], [psB])
                rs, rsB = rsp.next()
                rt, rtB = rtp.next()
                self.rstd_from(ps[:], psB, rs[:], rsB[0], 1.0, rt[:], rtB[0])
                self.tt(dst[:, tsl], Wb[:, tsl], rs[:], ALU.mult, [WbB, rsB[0]], [dstB[tb]])

            def load_head(h):
                wt, wtB = wtp.next()
                for i in range(4):
                    c0 = 1536 + i * 512 + h * 128
                    self.wload(wt[:, :, i * 128:(i + 1) * 128], w_in[:, c0:c0 + 128].rearrange("(kc p) n -> p kc n", p=128), wtB[i])
                return wt, wtB

            nxt_w = load_head(0)
            for h in range(4):
                wt, wtB = nxt_w
                for typ, dst, dstB in ((0, qT, qTB), (1, kT, kTB), (2, vT, vTB)):
                    for tb in range(NB):
                        ps, psB = self.psn()
                        self.proj(ps, psB, wt, wtB[typ], typ * 128, tb)
                        self.cp(ACT, Wa[:, 4 + tb * 512:4 + (tb + 1) * 512], ps[:], [psB], [WaB])
                    ci = typ * 4 + h
                    for j in range(4):
                        self.ts(dgc[:, j, :], self.ident_bf[:], sm[:, SM_GCONV + ci * 4 + j:SM_GCONV + ci * 4 + j + 1], None,
                                ALU.mult, None, [cB, WaB, dgcB], [dgcB])
                    for tb in range(NB):
                        tsl = slice(tb * 512, (tb + 1) * 512)
                        pc_, pcB = self.psn()
                        for j in range(4):
                            self.mm(pc_[:], dgc[:, j, :], Wa[:, 1 + tb * 512 + j:1 + tb * 512 + j + 512], j == 0, j == 3,
                                    [dgcB, WaB], [pcB])
                        if typ == 2:
                            self.act(vT[:, tsl], pc_[:], AF.Silu, [pcB], [vTB[tb]])
                        else:
                            self.act(Wb[:, tsl], pc_[:], AF.Silu, [pcB], [WbB])
                    if typ != 2:
                        for tb in range(NB):
                            l2n_block(tb, dst, dstB)
                if self.dbg == 5:
                    return
                for src, srcB, dst, dstB in ((kT, kTB, ktok, ktokB), (vT, vTB, vtok, vtokB)):
                    for half in range(2):
                        for j in range(8):
                            tt = half * 8 + j
                            self.P.op(PE, lambda e, o=self.psbf[:, j * 128:(j + 1) * 128], i_=src[:, tt * 128:(tt + 1) * 128]:
                                      e.transpose(out=o, in_=i_, identity=self.ident_bf[:]), [srcB[tt // 4], cB], [self.psbfB])
                        self.cp(DVE if half else ACT, dst[:, half * 8:half * 8 + 8, :],
                                self.psbf[:].rearrange("p (t e) -> p t e", e=128), [self.psbfB], [dstB[half]])
                if self.dbg == 6:
                    return
                self.memset(DVE, S32[:], 0.0, [S32B])
                self.memset(DVE, Sb[:], 0.0, [SbB])
                def unit_intra(n, sl, res):
                    tl = slice(n * 128, (n + 1) * 128)
                    tb = n // 4
                    col = lambda a: a[:, n, h:h + 1]
                    f32p, xyp, pp, up = sl["f32p"], sl["xyp"], sl["pp"], sl["up"]
                    for pl_ in [f32p, xyp, pp]:
                        pl_.i = 0
                    for nm in up:
                        up[nm].i = sl["upi"][nm]
                        sl["upi"][nm] ^= 1
                    bk = {"i": 0}

                    def psn_():
                        j = sl["banks"][bk["i"]]
                        bk["i"] = (bk["i"] + 1) % 2
                        return self.ps[j], self.psB[j]
                    dg, dgB = f32p.next()
                    self.ts(dg[:], cst[:, C_ID:C_ID + 128], col(gc), None, ALU.mult, None, [cB, gcB], dgB)
                    pr, prB = psn_()
                    self.mm(pr[:, 0:128], self.ones32[:], dg[:], True, True, [dgB[0], cB], [prB])
                    yield
                    aU, aUB = f32p.next()
                    aL, aLB = f32p.next()
                    eR, eRB = f32p.next()
                    self.cp(ACT, dg[:], pr[:, 0:128], [prB, dgB[0]], dgB)
                    yield
                    self.stt(aL[:], dg[:], col(gc), cst[:, C_POSL:C_POSL + 128], ALU.subtract, ALU.add, [dgB[0], gcB, cB], aLB)
                    self.stt(aU[:], dg[:], col(gc), cst[:, C_NEGU:C_NEGU + 128], ALU.subtract, ALU.add, [dgB[0], gcB, cB], aUB)
                    self.act(eR[:], dg[:], AF.Exp, dgB, eRB)
                    yield
                    self.act(aL[:], aL[:], AF.Exp, aLB, aLB, scale=-1.0)
                    self.act(aU[:], aU[:], AF.Exp, aUB, aUB)
                    pg, pgB = psn_()
                    self.mm(pg[:, 0:128], kT[:, tl], kT[:, tl], True, True, [kTB[tb]], [pgB])
                    yield
                    A32, A32B = f32p.next()
                    self.stt(A32[:], pg[:, 0:128], col(nbeta), aL[:], ALU.mult, ALU.mult, [pgB, nbetaB, aLB[0]], A32B)
                    yield
                    xy, xyB = xyp.next()
                    self.cp(ACT, xy[:, 0:128], A32[:], A32B, xyB)
                    pt, ptB = psn_()
                    self.tr(pt[:, 0:128], A32[:], A32B, [ptB])
                    yield
                    self.cp(ACT, xy[:, 128:256], pt[:, 0:128], [ptB, xyB[0]], xyB)
                    yield
                    Pk, PkB = pp.next()
                    self.tt(Pk[:], xy[:, 128:256], self.ident_bf[:], ALU.add, [xyB[0], cB], PkB)
                    for lv in range(1, 7):
                        px, pxB = psn_()
                        X, Y = xy[:, 0:128], xy[:, 128:256]
                        self.mm(px[:, 0:128], Y, X, True, True, xyB, [pxB])
                        if lv < 6:
                            self.mm(px[:, 128:256], X, Y, True, True, xyB, [pxB])
                        yield
                        xy2, xy2B = xyp.next()
                        wdt = 256 if lv < 6 else 128
                        self.cp(ACT, xy2[:, 0:wdt], px[:, 0:wdt], [pxB], xy2B)
                        yield
                        p2, p2B = psn_()
                        self.mm(p2[:, 0:128], xy2[:, 0:128], Pk[:], True, True, [xy2B[0], PkB[0]], [p2B])
                        yield
                        if lv < 6:
                            Pn, PnB = pp.next()
                        else:
                            Pn, PnB = up["TT"].next()
                        self.tt(Pn[:], Pk[:], p2[:, 0:128], ALU.add, [PkB[0], p2B], PnB)
                        Pk, PkB = Pn, PnB
                        xy, xyB = xy2, xy2B
                    TT, TTB = Pk, PkB
                    nkbg, nkbgB = up["nkbg"].next()
                    vb, vbB = up["vb"].next()
                    kdec, kdecB = up["kdec"].next()
                    self.ts(nkbg[:], ktok[:, n, :], col(nbeg), None, ALU.mult, None, [ktokB[n // 8], nbegB], nkbgB)
                    self.ts(vb[:], vtok[:, n, :], col(beta), None, ALU.mult, None, [vtokB[n // 8], betaB], vbB)
                    self.ts(kdec[:], ktok[:, n, :], col(ekd), None, ALU.mult, None, [ktokB[n // 8], ekdB], kdecB)
                    yield
                    pw, pwB = psn_()
                    self.mm(pw[:, 0:128], nkbg[:], TT[:], True, True, [nkbgB[0], TTB[0]], [pwB])
                    pq, pqB = psn_()
                    self.mm(pq[:, 0:128], kT[:, tl], qT[:, tl], True, True, [kTB[tb], qTB[tb]], [pqB])
                    yield
                    wTn, wTnB = up["wTn"].next()
                    self.cp(ACT, wTn[:], pw[:, 0:128], [pwB], wTnB)
                    qkT, qkTB = up["qkT"].next()
                    self.stt(qkT[:], pq[:, 0:128], SC, aU[:], ALU.mult, ALU.mult, [pqB, aUB[0]], qkTB)
                    qgT, qgTB = up["qgT"].next()
                    self.stt(qgT[:], eR[:], SC, qT[:, tl], ALU.mult, ALU.mult, [eRB[0], qTB[tb]], qgTB)
                    res[n] = dict(TT=(TT, TTB), vb=(vb, vbB), kdec=(kdec, kdecB), wTn=(wTn, wTnB), qkT=(qkT, qkTB), qgT=(qgT, qgTB))
                    yield

                def recur(ns, res):
                    b6, b6B = self.ps[6], self.psB[6]
                    for n in ns:
                        tl = slice(n * 128, (n + 1) * 128)
                        col = lambda a: a[:, n, h:h + 1]
                        r = res[n]
                        TT, TTB = r["TT"]
                        vb, vbB = r["vb"]
                        kdec, kdecB = r["kdec"]
                        wTn, wTnB = r["wTn"]
                        qkT, qkTB = r["qkT"]
                        qgT, qgTB = r["qgT"]
                        self.mm(b6[:, 0:128], TT[:], vb[:], True, False, [TTB[0], vbB[0]], [b6B])
                        self.mm(b6[:, 0:128], wTn[:], Sb[:], False, True, [wTnB[0], SbB], [b6B])
                        yield
                        vnew, vnewB = vnp.next()
                        self.cp(ACT, vnew[:], b6[:, 0:128], [b6B], vnewB)
                        yield
                        self.mm(b6[:, 128:256], Sb[:], qgT[:], True, False, [SbB, qgTB[0]], [b6B])
                        self.mm(b6[:, 128:256], vnew[:], qkT[:], False, True, [vnewB[0], qkTB[0]], [b6B])
                        self.mm(b6[:, 256:384], kdec[:], vnew[:], True, True, [kdecB[0], vnewB[0]], [b6B])
                        yield
                        self.cp(DVE, oT[:, tl], b6[:, 128:256], [b6B], [oTB[n]])
                        self.stt(S32[:], S32[:], col(eglast), b6[:, 256:384], ALU.mult, ALU.add, [S32B, eglB, b6B], [S32B])
                        yield
                        self.cp(ACT, Sb[:], S32[:], [S32B], [SbB])
                        yield

                def drive(gens):
                    gens = list(gens)
                    while gens:
                        nxt_ = []
                        for g_ in gens:
                            try:
                                next(g_)
                                nxt_.append(g_)
                            except StopIteration:
                                pass
                        gens = nxt_

                res = {}
                prev_rec = None
                for n0 in range(0, NT, KU):
                    ns = list(range(n0, min(n0 + KU, NT)))
                    gens = [unit_intra(n, slots[i_], res) for i_, n in enumerate(ns)]
                    if prev_rec is not None:
                        gens.append(prev_rec)
                    drive(gens)
                    prev_rec = recur(ns, res)
                drive([prev_rec])
                for tb in range(NB):
                    tsl = slice(tb * 512, (tb + 1) * 512)
                    oB_ = [WbB]
                    self.act(sqb[:, tsl], oT[:, tsl], AF.Square, oB_, [sqbB[tb]])
                    ps, psB = self.psn()
                    self.mm(ps[:], self.ones_bf[:], sqb[:, tsl], True, True, [sqbB[tb], cB], [psB])
                    rs, rsB = rsp.next()
                    rt, rtB = rtp.next()
                    self.rstd_from(ps[:], psB, rs[:], rsB[0], 1.0 / 128, rt[:], rtB[0])
                    pg, pgB = self.psn()
                    self.proj(pg, pgB, wt, wtB[3], 3 * 128, tb)
                    self.act(rt[:], pg[:], AF.Silu, [pgB], rtB)
                    self.stt(rs[:], oT[:, tsl], sm[:, SM_GDNG:SM_GDNG + 1], rs[:], ALU.mult, ALU.mult, oB_ + [rsB[0], cB], rsB)
                    self.tt(self.yT[:, h, tsl], rs[:], rt[:], ALU.mult, [rsB[0], rtB[0]], [self.yB[h][tb]])
                if h < 3:
                    nxt_w = load_head(h + 1)

    def ffn0(self):
        self.rmsnorm(1)
        with self.scope() as sc:
            self.swiglu_multi([(self.w["ffn_w_gate_up"], self.w["ffn_w_down"], D_FF, None)], self.ffn_pools(sc))

    def ple(self, b, i):
        self.rmsnorm(2 + 3 * i)
        with self.scope() as sc:
            pT = sc.sb("pT", [128, 2, S], BF16)
            pTB = bufs(NB, "pT")
            pin = RPool(sc, "pin", 3, [128, 256], F32)
            wg0 = sc.sb("wg0", [128, KC, 512], BF16)
            wg1 = sc.sb("wg1", [128, KC, 512], BF16)
            wpj = sc.sb("wpj", [128, 2, D], BF16)
            wg0B, wg1B, wpjB = Buf(), Buf(), Buf()
            wgd = self.w["ple_w_gate"][i]
            self.wload(wg0[:], wgd[:, 0:512].rearrange("(kc p) n -> p kc n", p=128), wg0B)
            self.wload(wg1[:], wgd[:, 512:1024].rearrange("(kc p) n -> p kc n", p=128), wg1B)
            self.wload(wpj[:], self.w["ple_w_proj"][i].rearrange("(kc p) n -> p kc n", p=128), wpjB)
            for tt in range(NT):
                pt_, ptB = pin.next()
                self.P.dma(SP, pt_[:], self.p[i, b, tt * 128:(tt + 1) * 128, :], [], ptB)
                ps, psB = self.psn()
                for j in range(2):
                    self.tr(ps[:, j * 128:(j + 1) * 128], pt_[:, j * 128:(j + 1) * 128], ptB, [psB])
                self.cp(ACT, pT[:, :, tt * 128:(tt + 1) * 128], ps[:, 0:256].rearrange("p (j t) -> p j t", j=2), [psB], [pTB[tt // 4]])
            sgp = RPool(sc, "psg", 3, [128, 512], F32)
            for d in range(KC):
                wg, wgB = (wg0, wg0B) if d < 4 else (wg1, wg1B)
                for tb in range(NB):
                    tsl = slice(tb * 512, (tb + 1) * 512)
                    pg, pgB = self.psn()
                    self.proj(pg, pgB, wg, wgB, (d % 4) * 128, tb)
                    pp_, ppB = self.psn()
                    for kc in range(2):
                        self.mm(pp_[:], wpj[:, kc, d * 128:(d + 1) * 128], pT[:, kc, tsl], kc == 0, kc == 1, [wpjB, pTB[tb]], [ppB])
                    sg, sgB = sgp.next()
                    self.act(sg[:], pg[:], AF.Sigmoid, [pgB], sgB)
                    self.tt(sg[:], sg[:], pp_[:], ALU.mult, [sgB[0], ppB], sgB)
                    self.tt(self.hT[:, d, tsl], self.hT[:, d, tsl], sg[:], ALU.add, [sgB[0], self.hB[d][tb]], [self.hB[d][tb]], eng=POOL)

    def mixer1(self):
        w_in = self.w["od_w_in"]
        sm = self.sm
        cB = self.cB
        self.rmsnorm(3)
        with self.scope() as sc:
            qT = sc.sb("aq", [128, 4, S], BF16)
            kT = sc.sb("ak", [128, 4, S], BF16)
            qB = [bufs(NB, "aq") for _ in range(4)]
            kB = [bufs(NB, "ak") for _ in range(4)]
            vtok = sc.sb("av", [128, NT, 512], BF16)
            vB = bufs(NT, "av")
            wtp = RPool(sc, "aw", 2, [128, KC, 512], BF16)
            for typ, dst, dstB, scl in ((0, qT, qB, 0.125), (1, kT, kB, 1.0)):
                wt, wtB = wtp.next()
                self.wload(wt[:], w_in[:, typ * 512:(typ + 1) * 512].rearrange("(kc p) n -> p kc n", p=128), wtB[0])
                for h in range(4):
                    for tb in range(NB):
                        ps, psB = self.psn()
                        self.proj(ps, psB, wt, wtB[0], h * 128, tb)
                        self.act(dst[:, h, tb * 512:(tb + 1) * 512], ps[:], AF.Copy, [psB], [dstB[h][tb]], scale=scl)
            wt, wtB = wtp.next()
            self.wload(wt[:], w_in[:, 1024:1536].rearrange("(kc p) n -> p kc n", p=128), wtB[0])
            for tt in range(NT):
                ps, psB = self.psn()
                for kc in range(KC):
                    self.mm(ps[:], self.hnT[:, kc, tt * 128:(tt + 1) * 128], wt[:, kc, :], kc == 0, kc == KC - 1,
                            [self.hnB[tt // 4], wtB[0]], [psB])
                self.cp(DVE if tt % 2 else ACT, vtok[:, tt, :], ps[:], [psB], [vB[tt]])
            ptp = RPool(sc, "pT", 6, [128, 512], BF16)
            tmp = RPool(sc, "atmp", 3, [128, 256], F32)
            rcp = RPool(sc, "arc", 4, [128, 512], F32)
            sqp = RPool(sc, "asq", 2, [128, 512], BF16)
            NS_ = 3
            LA = 2
            st_ = {"si": 0}

            def next_s():
                i = st_["si"]
                st_["si"] = (i + 1) % NS_
                return self.ps[i], self.psB[i]

            tiles = []
            for h in range(4):
                for qc in range(NB):
                    nkt = 4 * qc + 4
                    for kt in range(nkt):
                        for m in range(2):
                            tiles.append((h, qc, kt, m, nkt))

            def stageA(h, qc, kt, m, nkt):
                c31 = sm[:, SM_TBL + 31 * 4 + h:SM_TBL + 31 * 4 + h + 1]
                d = kt - 4 * qc
                qlo = max(0, d) * 128
                pss, pssB = next_s()
                prt = slice(m * 64, (m + 1) * 64)
                self.mm(pss[:, qlo:512], kT[prt, h, kt * 128:(kt + 1) * 128], qT[prt, h, qc * 512 + qlo:(qc + 1) * 512],
                        True, True, [kB[h][kt // 4], qB[h][qc]], [pssB])
                pT_, pTB = ptp.next()
                if d >= 0:
                    n0, n1, b0 = qlo, min(qlo + 256, 512), 0
                elif d == -1:
                    n0, n1, b0 = 0, 128, 128
                else:
                    n0 = n1 = b0 = 0
                if n1 > n0:
                    t_, tB = tmp.next()
                    w_ = n1 - n0
                    self.tt(t_[:, 0:w_], pss[:, n0:n1], self.Bh[:, h, b0:b0 + w_], ALU.add, [pssB, cB], tB)
                    self.act(pT_[:, n0:n1], t_[:, 0:w_], AF.Exp, tB, pTB)
                if n1 < 512:
                    f0 = max(n1, qlo)
                    self.act(pT_[:, f0:512], pss[:, f0:512], AF.Exp, [pssB, cB], pTB, bias=c31)
                return pT_, pTB, qlo

            def stageB(tile, info):
                h, qc, kt, m, nkt = tile
                pT_, pTB, qlo = info
                self.mm(self.ps[3 + m][:, qlo:512], vtok[:, kt, h * 128:(h + 1) * 128], pT_[:, qlo:512],
                        kt == 0, kt == nkt - 1, [vB[kt], pTB[0]], [self.psB[3 + m]])
                self.mm(self.ps[5 + m][:, qlo:512], self.ones_bf[:], pT_[:, qlo:512],
                        kt == 0, kt == nkt - 1, [cB, pTB[0]], [self.psB[5 + m]])
                if kt == nkt - 1 and m == 1:
                    r1, r1B = rcp.next()
                    r2, r2B = rcp.next()
                    self.recip(r1[:], self.ps[5][:], [self.psB[5]], r1B)
                    self.recip(r2[:], self.ps[6][:], [self.psB[6]], r2B)
                    self.tt(r1[:], r1[:], self.ps[3][:], ALU.mult, [r1B[0], self.psB[3]], r1B)
                    self.stt(r2[:], r2[:], self.neglam[:, 0:1], self.ps[4][:], ALU.mult, ALU.mult, [r2B[0], self.psB[4], cB], r2B)
                    self.tt(r1[:], r1[:], r2[:], ALU.add, [r1B[0], r2B[0]], r1B)
                    sq, sqB = sqp.next()
                    self.act(sq[:], r1[:], AF.Square, r1B, sqB)
                    pn, pnB = next_s()
                    self.mm(pn[:], self.ones_bf[:], sq[:], True, True, sqB + [cB], [pnB])
                    self.rstd_from(pn[:], pnB, r2[:], r2B[0], 1.0 / 128, r2[:], r2B[0])
                    self.stt(self.yT[:, h, qc * 512:(qc + 1) * 512], r1[:], self.dgl[:, 0:1], r2[:], ALU.mult, ALU.mult,
                             [r1B[0], r2B[0], cB], [self.yB[h][qc]])

            infos = {}
            for i in range(min(LA, len(tiles))):
                infos[i] = stageA(*tiles[i])
            for i in range(len(tiles)):
                if i + LA < len(tiles):
                    infos[i + LA] = stageA(*tiles[i + LA])
                stageB(tiles[i], infos.pop(i))
        self.outproj(self.w["od_w_out"], 0)
        with self.scope() as sc:
            wa = sc.sb("cwa", [128, KC, 512], BF16)
            wb = sc.sb("cwb", [128, KC, 512], BF16)
            waB, wbB = Buf(), Buf()
            self.wload(wa[:], w_in[:, 1536:2048].rearrange("(kc p) n -> p kc n", p=128), waB)
            self.wload(wb[:], w_in[:, 2048:2560].rearrange("(kc p) n -> p kc n", p=128), wbB)
            Wc = RPool(sc, "Wc", 1, [128, 32 + S], BF16)
            for t, tB in zip(Wc.tiles, Wc.bufs):
                self.memset(DVE, t[:, 0:32], 0.0, tB)
            cc = sc.sb("cc", [128, 4, S], F32)
            ccB = [bufs(NB, "cc") for _ in range(4)]
            dgp = RPool(sc, "dgm", 1, [128, 31, 128], BF16)
            sgp = RPool(sc, "csg", 2, [128, 512], F32)
            for c in range(4):
                wc, wcB = Wc.next()
                for tb in range(NB):
                    pa, paB = self.psn()
                    self.proj(pa, paB, wa, waB, c * 128, tb)
                    pb, pbB = self.psn()
                    self.proj(pb, pbB, wb, wbB, c * 128, tb)
                    sg, sgB = sgp.next()
                    self.act(sg[:], pb[:], AF.Sigmoid, [pbB], sgB)
                    self.tt(wc[:, 32 + tb * 512:32 + (tb + 1) * 512], pa[:], sg[:], ALU.mult, [paB, sgB[0]], wcB)
                dg, dgB = dgp.next()
                for j in range(31):
                    self.ts(dg[:, j, :], self.ident_bf[:], sm[:, SM_DWW + c * 31 + j:SM_DWW + c * 31 + j + 1], None, ALU.mult, None,
                            [cB, wcB[0]], dgB)
                for tb in range(NB):
                    pc_, pcB = self.psn()
                    for j in range(31):
                        self.mm(pc_[:], dg[:, j, :], wc[:, 2 + tb * 512 + j:2 + tb * 512 + j + 512], j == 0, j == 30,
                                [dgB[0], wcB[0]], [pcB])
                    self.act(cc[:, c, tb * 512:(tb + 1) * 512], pc_[:], AF.Identity, [pcB, cB], [ccB[c][tb]],
                             bias=sm[:, SM_DWB + c:SM_DWB + c + 1])
            rsp = RPool(sc, "crs", 1, [128, 512], F32)
            rtp = RPool(sc, "crt", 1, [128, 512], F32)
            sqp = RPool(sc, "csq", 1, [128, 4, 512], BF16)
            for tb in range(NB):
                tsl = slice(tb * 512, (tb + 1) * 512)
                cBs = [ccB[c][tb] for c in range(4)]
                pm, pmB = self.psn()
                for c in range(4):
                    self.mm(pm[:], self.ones32[:], cc[:, c, tsl], c == 0, c == 3, [ccB[c][tb], cB], [pmB])
                rt, rtB = rtp.next()
                self.ts(rt[:], pm[:], 1.0 / 512, None, ALU.mult, None, [pmB], rtB)
                for c in range(4):
                    self.tt(cc[:, c, tsl], cc[:, c, tsl], rt[:], ALU.subtract, [ccB[c][tb], rtB[0]], [ccB[c][tb]])
                sq, sqB = sqp.next()
                self.act(sq[:], cc[:, :, tsl], AF.Square, cBs, sqB)
                pv, pvB = self.psn()
                for c in range(4):
                    self.mm(pv[:], self.ones_bf[:], sq[:, c, :], c == 0, c == 3, sqB + [cB], [pvB])
                rs, rsB = rsp.next()
                self.rstd_from(pv[:], pvB, rs[:], rsB[0], 1.0 / 512, rt[:], rtB[0])
                for c in range(4):
                    self.tt(cc[:, c, tsl], cc[:, c, tsl], rs[:], ALU.mult, [ccB[c][tb], rsB[0]], [ccB[c][tb]])
                    self.act(self.yT[:, c, tsl], cc[:, c, tsl], AF.Silu, [ccB[c][tb], cB], [self.yB[c][tb]],
                             bias=sm[:, SM_LNB + c:SM_LNB + c + 1], scale=sm[:, SM_LNG + c:SM_LNG + c + 1])
        self.outproj(self.w["od_w_out"], 512)

    def moe(self):
        sm = self.sm
        cB = self.cB
        with self.scope() as sc0:
            lg = sc0.sb("lg", [128, NT, 8], F32)
            lgB = Buf()
            GT = sc0.sb("GT", [8, S], F32)
            GTB = Buf()
            gate = sc0.sb("gate", [128, S], F32)
            gateB = Buf()
            sel = sc0.sb("sel", [8, NEXP * 128], F32)
            selB = Buf()
            self.P.dma(SP, sel[:], self.w["sel"][:, :], [], [selB])

            ss = sc0.sb("mss", [128, NT], F32)
            T = {n: sc0.sb(n, [128, NT, 8], F32) for n in ["eq1", "lg2", "eq2", "Gd"]}
            V = {n: sc0.sb(n, [128, NT], F32) for n in ["m1", "m2", "g1", "g2"]}
            GTBs = bufs(NB, "GT")

            def hook(tb, sq, sqB):
                sl = slice(tb * 4, (tb + 1) * 4)
                B_ = {n: Buf(n) for n in ["ss", "lg", "eq1", "lg2", "eq2", "Gd", "m1", "m2", "g1", "g2"]}
                ps, psB = self.psn()
                for t4 in range(4):
                    for c in range(KC):
                        self.mm(ps[:, t4:t4 + 1], sq[:, c, t4 * 128:(t4 + 1) * 128], self.ones_bf[:, 0:1], c == 0, c == KC - 1,
                                sqB + [cB], [psB])
                for t4 in range(4):
                    tt = tb * 4 + t4
                    for kc in range(KC):
                        self.mm(ps[:, 8 + t4 * 8:16 + t4 * 8], self.hT[:, kc, tt * 128:(tt + 1) * 128], self.gwr[:, kc, :],
                                kc == 0, kc == KC - 1, [self.hB[kc][tb], cB], [psB])
                self.ts(ss[:, sl], ps[:, 0:4], 1.0 / D, EPS, ALU.mult, ALU.add, [psB], [B_["ss"]])
                self.act(ss[:, sl], ss[:, sl], AF.Sqrt, [B_["ss"]], [B_["ss"]])
                self.recip(ss[:, sl], ss[:, sl], [B_["ss"]], [B_["ss"]])
                bc = lambda a: a[:, sl].unsqueeze(2).to_broadcast([128, 4, 8])
                lgs = lg[:, sl, :]
                self.tt(lgs, ps[:, 8:40].rearrange("p (t e) -> p t e", e=8), bc(ss), ALU.mult, [psB, B_["ss"]], [B_["lg"]])
                m1, m2, g1, g2 = V["m1"], V["m2"], V["g1"], V["g2"]
                eq1, lg2, eq2, Gd = T["eq1"][:, sl, :], T["lg2"][:, sl, :], T["eq2"][:, sl, :], T["Gd"][:, sl, :]
                self.P.op(DVE, lambda e: e.tensor_reduce(out=m1[:, sl], in_=lgs, axis=AX.X, op=ALU.max), [B_["lg"]], [B_["m1"]])
                self.tt(eq1, lgs, bc(m1), ALU.is_equal, [B_["lg"], B_["m1"]], [B_["eq1"]])
                self.stt(lg2, eq1, -1e9, lgs, ALU.mult, ALU.add, [B_["eq1"], B_["lg"]], [B_["lg2"]])
                self.P.op(DVE, lambda e: e.tensor_reduce(out=m2[:, sl], in_=lg2, axis=AX.X, op=ALU.max), [B_["lg2"]], [B_["m2"]])
                self.tt(eq2, lg2, bc(m2), ALU.is_equal, [B_["lg2"], B_["m2"]], [B_["eq2"]])
                self.tt(g1[:, sl], m1[:, sl], m2[:, sl], ALU.subtract, [B_["m1"], B_["m2"]], [B_["g1"]])
                self.act(g1[:, sl], g1[:, sl], AF.Sigmoid, [B_["g1"]], [B_["g1"]])
                self.ts(g2[:, sl], g1[:, sl], -1.0, 1.0, ALU.mult, ALU.add, [B_["g1"]], [B_["g2"]])
                self.tt(eq1, eq1, bc(g1), ALU.mult, [B_["eq1"], B_["g1"]], [B_["eq1"]])
                self.tt(eq2, eq2, bc(g2), ALU.mult, [B_["eq2"], B_["g2"]], [B_["eq2"]])
                self.tt(Gd, eq1, eq2, ALU.add, [B_["eq1"], B_["eq2"]], [B_["Gd"]])
                pt_, ptB = self.psn()
                for j in range(4):
                    self.tr(pt_[0:8, j * 128:(j + 1) * 128], T["Gd"][:, tb * 4 + j, :], [B_["Gd"]], [ptB])
                self.cp(ACT, GT[:, tb * 512:(tb + 1) * 512], pt_[0:8, :], [ptB], [GTBs[tb]])

            self.rmsnorm(4, cb_sq=hook)
            with self.scope() as sc:
                pools = self.ffn_pools(sc)

                def mk_gate(ex):
                    def f():
                        for tb in range(NB):
                            tsl = slice(tb * 512, (tb + 1) * 512)
                            ps, psB = self.psn()
                            self.mm(ps[:], sel[:, ex * 128:(ex + 1) * 128], GT[:, tsl], True, True, [selB, GTBs[tb]], [psB])
                            self.cp(ACT, gate[:, tsl], ps[:], [psB], [gateB])
                        return gate, gateB
                    return f
                jobs = [(self.w["moe_w_gate_up"][ex], self.w["moe_w_down"][ex], D_FFE, mk_gate(ex)) for ex in range(NEXP)]
                self.swiglu_multi(jobs, pools)

    def setup(self, es, dr):
        nc = self.nc
        sb = lambda n, s, d: es.enter_context(nc.sbuf_tensor("sb_" + n, s, d))
        self.cB = Buf("const")
        cB = self.cB
        self.hT = sb("hT", [128, KC, S], F32)
        self.hnT = sb("hnT", [128, KC, S], BF16)
        self.yT = sb("yT", [128, 4, S], BF16)
        self.hB = [bufs(NB, f"h{c}_") for c in range(KC)]
        self.hnB = bufs(NB, "hn")
        self.yB = [bufs(NB, f"y{c}_") for c in range(4)]
        self.sm = sb("sm", [128, SM_N], F32)
        self.cst = sb("cst", [128, C_N], F32)
        self.ones_bf = sb("ones_bf", [128, 128], BF16)
        self.ident_bf = sb("ident_bf", [128, 128], BF16)
        self.ones32 = sb("ones32", [128, 128], F32)
        self.Bh = sb("Bh", [128, 4, 256], F32)
        self.negA = sb("negA", [128, 4], F32)
        self.gwr = sb("gwr", [128, KC, 8], F32)
        self.neglam = sb("neglam", [128, 1], F32)
        self.dgl = sb("dgl", [128, 1], F32)
        self.ident = self.cst[:, C_ID:C_ID + 128]
        self.ps = [es.enter_context(nc.psum_tensor(f"ps{i}", [128, 512], F32)) for i in range(7)]
        self.psB = bufs(7, "ps")
        self.psbf = es.enter_context(nc.psum_tensor("psbf", [128, 1024], BF16))
        self.psbfB = Buf("psbf")
        self.ps_rot = list(range(7))
        self.ps_i = 0
        self.x, self.p, self.out = dr["x"], dr["p"], dr["out"]
        self.outB = Buf("out")
        self.w = dr
        P = self.P
        P.dma(SP, self.sm[:], dr["small"][:, :], [], [cB])
        P.dma(SP, self.cst[:], dr["cst"][:, :], [], [cB])
        self.memset(DVE, self.ones_bf[:], 1.0, [cB])
        self.memset(DVE, self.ones32[:], 1.0, [cB])
        self.cp(DVE, self.ident_bf[:], self.cst[:, C_ID:C_ID + 128], [cB], [cB])
        sm = self.sm
        for kc in range(KC):
            self.ts(self.gwr[:, kc, :], sm[:, SM_RT + kc * 8:SM_RT + kc * 8 + 8], sm[:, SM_G + 4 * 8 + kc:SM_G + 4 * 8 + kc + 1],
                    None, ALU.mult, None, [cB], [cB])
        self.act(self.negA[:], sm[:, SM_ALOG:SM_ALOG + 4], AF.Exp, [cB], [cB])
        self.ts(self.negA[:], self.negA[:], -1.0, None, ALU.mult, None, [cB], [cB])
        with self.scope() as sc:
            pr = sc.sb("lpr", [128, 128], F32)
            s12 = sc.sb("ls", [128, 2], F32)
            tB = Buf()
            self.tt(pr[:, 0:64], sm[:, SM_LAM:SM_LAM + 64], sm[:, SM_LAM + 64:SM_LAM + 128], ALU.mult, [cB], [tB])
            self.tt(pr[:, 64:128], sm[:, SM_LAM + 128:SM_LAM + 192], sm[:, SM_LAM + 192:SM_LAM + 256], ALU.mult, [cB, tB], [tB])
            P.op(DVE, lambda e: e.tensor_reduce(out=s12[:], in_=pr[:].rearrange("p (a b) -> p a b", a=2), axis=AX.X, op=ALU.add), [tB], [tB])
            self.act(s12[:], s12[:], AF.Exp, [tB], [tB])
            self.tt(self.neglam[:], s12[:, 1:2], s12[:, 0:1], ALU.subtract, [tB], [cB])
            self.ts(self.neglam[:], self.neglam[:], -LAMBDA_INIT, None, ALU.add, None, [cB], [cB])
            self.ts(self.dgl[:], sm[:, SM_DIFFG:SM_DIFFG + 1], 1.0 - LAMBDA_INIT, None, ALU.mult, None, [cB], [cB])
            bm = sc.sb("bm", [128, 32, 256], F32)
            P.dma(SP, bm[:], dr["bmask"][:, :].rearrange("p (b j) -> p b j", b=32), [], [tB])
            for h in range(4):
                self.cp(DVE, self.Bh[:, h, :], self.cst[:, C_NEGM:C_NEGM + 256], [cB], [cB])
                for b in range(32):
                    self.stt(self.Bh[:, h, :], bm[:, b, :], sm[:, SM_TBL + b * 4 + h:SM_TBL + b * 4 + h + 1], self.Bh[:, h, :],
                             ALU.mult, ALU.add, [tB, cB], [cB])

    def sequence(self, b):
        st = self.stop_after
        self.load_x(b)
        stages = [self.mixer0, self.ffn0, lambda: self.ple(b, 0), self.mixer1, self.moe, lambda: self.ple(b, 1)]
        import os
        only = os.environ.get("KONLY")
        for i, f in enumerate(stages):
            if st is not None and i >= st:
                break
            if only is not None and str(i + 1) not in only.split(","):
                continue
            f()
        self.store_out(b, final=(st is None or st >= 7))


W_NAMES = ["ev_w_in", "ev_w_out", "od_w_in", "od_w_out", "ffn_w_gate_up", "ffn_w_down",
           "moe_w_gate_up", "moe_w_down", "ple_w_proj", "ple_w_gate"]
W_SHAPES = {"ev_w_in": [D, EV_IN], "ev_w_out": [D, D], "od_w_in": [D, OD_IN], "od_w_out": [D, D],
            "ffn_w_gate_up": [D, 2 * D_FF], "ffn_w_down": [D_FF, D],
            "moe_w_gate_up": [NEXP, D, 2 * D_FFE], "moe_w_down": [NEXP, D_FFE, D],
            "ple_w_proj": [2, 256, D], "ple_w_gate": [2, D, D]}


def build(nseq=SEQ_PER_CORE, stop_after=None):
    nc = bass.Bass("TRN2", target_bir_lowering=False)
    dr = {}
    dr["x"] = nc.dram_tensor("x", [nseq, S, D], F32, kind="ExternalInput").ap()
    dr["p"] = nc.dram_tensor("p", [2, nseq, S, 256], F32, kind="ExternalInput").ap()
    dr["small"] = nc.dram_tensor("small", [128, SM_N], F32, kind="ExternalInput").ap()
    dr["cst"] = nc.dram_tensor("cst", [128, C_N], F32, kind="ExternalInput").ap()
    dr["sel"] = nc.dram_tensor("sel", [8, NEXP * 128], F32, kind="ExternalInput").ap()
    dr["bmask"] = nc.dram_tensor("bmask", [128, 32 * 256], F32, kind="ExternalInput").ap()
    for n in W_NAMES:
        dr[n] = nc.dram_tensor(n, W_SHAPES[n], F32, kind="ExternalInput").ap()
    dr["out"] = nc.dram_tensor("out", [nseq, S, D], F32, kind="ExternalOutput").ap()
    k = K(nc, nseq, stop_after)
    with ExitStack() as es:
        sems = {e: es.enter_context(nc.semaphore("s_" + e)) for e in ENGS}
        dsems = [es.enter_context(nc.semaphore(f"d{i}")) for i in range(Prog.NDMA)]
        k.setup(es, dr)
        for b in range(nseq):
            k.sequence(b)
        k.P.barrier()
        block = es.enter_context(nc.Block())
        k.P.emit(block, sems, dsems)
    return nc


def _rel_bucket(n):
    n = np.maximum(n, 0)
    nf = np.maximum(n, 1).astype(np.float32)
    large = 16 + (np.log(nf / np.float32(16)) / np.float32(math.log(128 / 16)) * np.float32(16)).astype(np.int32)
    large = np.minimum(large, 31)
    return np.where(n < 16, n, large)


def host_consts():
    i = np.arange(128)[:, None]
    j = np.arange(128)[None, :]
    cst = np.zeros((128, C_N), np.float32)
    cst[:, C_ID:C_ID + 128] = np.eye(128, dtype=np.float32)
    cst[:, C_U:C_U + 128] = (i <= j)
    cst[:, C_NEGU:C_NEGU + 128] = np.where(j >= i, 0.0, NEGBIG)
    cst[:, C_POSL:C_POSL + 128] = np.where(j < i, 0.0, -NEGBIG)
    j2 = np.arange(256)[None, :]
    cst[:, C_NEGM:C_NEGM + 256] = np.where(j2 >= i, 0.0, NEGBIG)
    sel = np.zeros((8, NEXP, 128), np.float32)
    for e in range(NEXP):
        sel[e, e, :] = 1.0
    rel = j2 - i
    bk = _rel_bucket(rel)
    bm = np.zeros((128, 32, 256), np.float32)
    for b in range(32):
        bm[:, b, :] = (bk == b) & (rel >= 0)
    return cst, sel.reshape(8, NEXP * 128), bm.reshape(128, 32 * 256)


def pack_small(inp):
    sm = np.zeros((128, SM_N), np.float32)
    gains = [inp["norm_mix_g"][0], inp["norm_ffn_g"][0], inp["norm_ple_g"][0],
             inp["norm_mix_g"][1], inp["norm_ffn_g"][1], inp["norm_ple_g"][1], inp["final_norm_g"]]
    for gi, g in enumerate(gains):
        sm[:, SM_G + gi * 8:SM_G + gi * 8 + 8] = np.asarray(g).reshape(8, 128).T
    sm[:, SM_CONVA:SM_CONVA + 12] = inp["ev_conv_a"][0].reshape(3, 4, 128).transpose(2, 1, 0).reshape(128, 12)
    sm[:, SM_GCONV:SM_GCONV + 48] = inp["ev_gdn_conv"][0].reshape(4, 12, 128).transpose(2, 1, 0).reshape(128, 48)
    sm[:, SM_GDNG] = inp["ev_gdn_norm_g"][0]
    sm[:, SM_DIFFG] = inp["od_diff_norm_g"][0]
    sm[:, SM_DWW:SM_DWW + 124] = inp["od_conf_dw_w"][0].reshape(31, 4, 128).transpose(2, 1, 0).reshape(128, 124)
    sm[:, SM_DWB:SM_DWB + 4] = inp["od_conf_dw_b"][0].reshape(4, 128).T
    sm[:, SM_LNG:SM_LNG + 4] = inp["od_conf_ln_g"][0].reshape(4, 128).T
    sm[:, SM_LNB:SM_LNB + 4] = inp["od_conf_ln_b"][0].reshape(4, 128).T
    sm[:, SM_RT:SM_RT + 64] = inp["moe_router"][0].reshape(8, 128, 8).transpose(1, 0, 2).reshape(128, 64)
    sm[:, SM_ALOG:SM_ALOG + 4] = np.broadcast_to(inp["ev_gdn_A_log"][0], (128, 4))
    sm[:, SM_DTB:SM_DTB + 4] = np.broadcast_to(inp["ev_gdn_dt_bias"][0], (128, 4))
    sm[:, SM_LAM:SM_LAM + 256] = np.broadcast_to(inp["od_lambda"][0].reshape(256), (128, 256))
    sm[:, SM_TBL:SM_TBL + 128] = np.broadcast_to(inp["rel_bias"].reshape(128), (128, 128))
    return sm


def make_in_maps(inp, nseq, ncores, b0=0):
    cst, sel, bm = host_consts()
    sm = pack_small(inp)
    shared = {"small": sm, "cst": cst, "sel": sel, "bmask": bm,
              "ev_w_in": np.ascontiguousarray(inp["ev_w_in"][0]), "ev_w_out": np.ascontiguousarray(inp["ev_w_out"][0]),
              "od_w_in": np.ascontiguousarray(inp["od_w_in"][0]), "od_w_out": np.ascontiguousarray(inp["od_w_out"][0]),
              "ffn_w_gate_up": np.ascontiguousarray(inp["ffn_w_gate_up"][0]), "ffn_w_down": np.ascontiguousarray(inp["ffn_w_down"][0]),
              "moe_w_gate_up": np.ascontiguousarray(inp["moe_w_gate_up"][0]), "moe_w_down": np.ascontiguousarray(inp["moe_w_down"][0]),
              "ple_w_proj": np.ascontiguousarray(inp["ple_w_proj"]), "ple_w_gate": np.ascontiguousarray(inp["ple_w_gate"])}
    maps = []
    for c in range(ncores):
        m = dict(shared)
        lo = b0 + c * nseq
        m["x"] = np.ascontiguousarray(inp["x"][lo:lo + nseq])
        m["p"] = np.ascontiguousarray(inp["p"][:, lo:lo + nseq])
        maps.append(m)
    return maps


def kernel(**inputs):
    inp = {k: np.asarray(v) for k, v in inputs.items()}
    nc = build(SEQ_PER_CORE, None)
    maps = make_in_maps(inp, SEQ_PER_CORE, NCORES)
    res = run_bass_kernel_spmd(nc, maps, core_ids=list(range(NCORES)))
    return np.concatenate([r["out"] for r in res.results], axis=0).astype(np.float32)
```

```python
import math
from contextlib import ExitStack

import numpy as np
import concourse.bass as bass
import concourse.mybir as mybir
from concourse.bass_utils import run_bass_kernel_spmd

F32 = mybir.dt.float32
BF16 = mybir.dt.bfloat16
AF = mybir.ActivationFunctionType
ALU = mybir.AluOpType
AX = mybir.AxisListType

PE, ACT, DVE, POOL, SP = "pe", "act", "dve", "pool", "sp"
ENGS = (PE, ACT, DVE, POOL, SP)

S = 2048
D = 1024
NT = 16
NB = 4
KC = 8
NCORES = 8
SEQ_PER_CORE = 4
EPS = 1e-6
EV_IN = 3592
OD_IN = 2560
D_FF = 2816
D_FFE = 3584
NEXP = 8
LAMBDA_INIT = 0.8 - 0.6 * math.exp(-0.3 * 1)
NEGBIG = -30000.0

SM_G = 0
SM_CONVA = 56
SM_GCONV = 68
SM_GDNG = 116
SM_DIFFG = 117
SM_DWW = 118
SM_DWB = 242
SM_LNG = 246
SM_LNB = 250
SM_RT = 254
SM_ALOG = 318
SM_DTB = 322
SM_LAM = 326
SM_TBL = 582
SM_N = 710
C_ID = 0
C_U = 128
C_NEGU = 256
C_POSL = 384
C_NEGM = 512
C_N = 768


import os as _os
PEPE = bool(int(_os.environ.get("KPEPE", "0")))
GDN_BARRIER = bool(int(_os.environ.get("KGDNBAR", "0")))


class Buf:
    __slots__ = ("name", "w", "r")

    def __init__(self, name=""):
        self.name = name
        self.w = None
        self.r = []


def bufs(n, name=""):
    return [Buf(f"{name}{i}") for i in range(n)]


class Prog:
    NDMA = 32

    def __init__(self, nc):
        self.nc = nc
        self.streams = {e: [] for e in ENGS}
        self.cnt = {e: 0 for e in ENGS}
        self.known = {e: {} for e in ENGS}
        self.dma_cnt = [0] * self.NDMA
        self.dma_rr = 0
        self.dma_rr2 = [0, 0]

    def _deps(self, eng, reads, writes):
        deps = {}

        def add(ev):
            if ev is None:
                return
            k, v = ev
            if k == PE and eng == PE and not PEPE:
                return
            if deps.get(k, 0) < v:
                deps[k] = v
        for b in reads:
            add(b.w)
        for b in writes:
            add(b.w)
            for ev in b.r:
                add(ev)
        kn = self.known[eng]
        out = []
        for k, v in deps.items():
            if kn.get(k, 0) < v:
                kn[k] = v
                out.append((k, v))
        return out

    def _mark(self, ev, reads, writes):
        for b in reads:
            b.r.append(ev)
            if len(b.r) > 64:
                m = {}
                for k, v in b.r:
                    if m.get(k, 0) < v:
                        m[k] = v
                b.r = list(m.items())
        for b in writes:
            b.w = ev
            b.r = []

    def op(self, eng, fn, reads=(), writes=()):
        waits = self._deps(eng, reads, writes)
        self.cnt[eng] += 1
        ev = (eng, self.cnt[eng])
        self.streams[eng].append(("op", fn, waits))
        self._mark(ev, reads, writes)
        return ev

    def dma(self, q, out, in_, reads=(), writes=()):
        waits = self._deps(q, reads, writes)
        g = 0 if q == POOL else 1
        half = self.NDMA // 2
        i = g * half + self.dma_rr2[g]
        self.dma_rr2[g] = (self.dma_rr2[g] + 1) % half
        key = ("dma", i)
        prev = self.dma_cnt[i]
        if prev and self.known[q].get(key, 0) < prev:
            self.known[q][key] = prev
            waits.append((key, prev))
        self.dma_cnt[i] = prev + 16
        ev = (key, prev + 16)
        self.streams[q].append(("dma", (out, in_, i), waits))
        self._mark(ev, reads, writes)
        return ev

    def barrier(self):
        for e in ENGS:
            waits = []
            kn = self.known[e]
            for f in ENGS:
                if f != e and self.cnt[f] > kn.get(f, 0):
                    kn[f] = self.cnt[f]
                    waits.append((f, self.cnt[f]))
            for i in range(self.NDMA):
                key = ("dma", i)
                if self.dma_cnt[i] > kn.get(key, 0):
                    kn[key] = self.dma_cnt[i]
                    waits.append((key, self.dma_cnt[i]))
            if waits:
                self.streams[e].append(("wait", None, waits))

    def emit(self, block, sems, dsems):
        def semof(k):
            return dsems[k[1]] if isinstance(k, tuple) else sems[k]

        def run(name, e):
            for kind, payload, waits in self.streams[name]:
                for k, v in waits:
                    e.wait_ge(semof(k), v)
                if kind == "op":
                    payload(e).then_inc(sems[name], 1)
                elif kind == "dma":
                    out, in_, i = payload
                    e.dma_start(out=out, in_=in_).then_inc(dsems[i], 16)

        @block.sync
        def _(e):
            run(SP, e)

        @block.scalar
        def _(e):
            run(ACT, e)

        @block.vector
        def _(e):
            run(DVE, e)

        @block.gpsimd
        def _(e):
            run(POOL, e)

        @block.tensor
        def _(e):
            run(PE, e)


class RPool:
    def __init__(self, sc, name, n, shape, dt, nb=1):
        self.tiles = [sc.sb(f"{name}{i}", shape, dt) for i in range(n)]
        self.bufs = [bufs(nb, f"{name}{i}_") for i in range(n)]
        self.i = 0

    def next(self):
        t, b = self.tiles[self.i], self.bufs[self.i]
        self.i = (self.i + 1) % len(self.tiles)
        return t, b


class Scope:
    def __init__(self, k):
        self.k = k
        self.es = ExitStack()

    def __enter__(self):
        self.es.__enter__()
        return self

    def __exit__(self, *a):
        self.k.P.barrier()
        return self.es.__exit__(*a)

    def sb(self, name, shape, dt):
        self.k.uid += 1
        return self.es.enter_context(self.k.nc.sbuf_tensor(f"{name}_{self.k.uid}", shape, dt))


class K:
    def __init__(self, nc, nseq, stop_after):
        self.nc = nc
        self.P = Prog(nc)
        self.uid = 0
        self.nseq = nseq
        self.stop_after = stop_after
        import os
        self.dbg = int(os.environ.get("KDBG", "0"))

    def mm(self, out, lhsT, rhs, start, stop, r, w):
        self.P.op(PE, lambda e: e.matmul(out, lhsT=lhsT, rhs=rhs, start=start, stop=stop), r, w)

    def tr(self, out, in_, r, w):
        idn = self.ident
        self.P.op(PE, lambda e: e.transpose(out=out, in_=in_, identity=idn), r + [self.cB], w)

    def act(self, out, in_, func, r, w, bias=None, scale=None):
        kw = {}
        if bias is not None:
            kw["bias"] = bias
        if scale is not None:
            kw["scale"] = scale
        self.P.op(ACT, lambda e: e.activation(out=out, in_=in_, func=func, **kw), r, w)

    def cp(self, eng, out, in_, r, w):
        if eng == ACT:
            self.P.op(ACT, lambda e: e.copy(out=out, in_=in_), r, w)
        else:
            self.P.op(eng, lambda e: e.tensor_copy(out=out, in_=in_), r, w)

    def tt(self, out, in0, in1, op, r, w, eng=DVE):
        self.P.op(eng, lambda e: e.tensor_tensor(out=out, in0=in0, in1=in1, op=op), r, w)

    def ts(self, out, in0, s1, s2, op0, op1, r, w, eng=DVE):
        if s2 is None:
            self.P.op(eng, lambda e: e.tensor_scalar(out=out, in0=in0, scalar1=s1, scalar2=None, op0=op0), r, w)
        else:
            self.P.op(eng, lambda e: e.tensor_scalar(out=out, in0=in0, scalar1=s1, scalar2=s2, op0=op0, op1=op1), r, w)

    def stt(self, out, in0, scalar, in1, op0, op1, r, w, eng=DVE):
        self.P.op(eng, lambda e: e.scalar_tensor_tensor(out=out, in0=in0, scalar=scalar, in1=in1, op0=op0, op1=op1), r, w)

    def recip(self, out, in_, r, w):
        self.P.op(DVE, lambda e: e.reciprocal(out=out, in_=in_), r, w)

    def memset(self, eng, ap, val, w):
        self.P.op(eng, lambda e: e.memset(ap, val), [], w)

    def psn(self):
        i = self.ps_i
        self.ps_i = (i + 1) % len(self.ps_rot)
        j = self.ps_rot[i]
        return self.ps[j], self.psB[j]

    def scope(self):
        return Scope(self)

    def wload(self, tile_ap, dram_ap, wbuf):
        self.P.dma(POOL, tile_ap, dram_ap, [], [wbuf])

    def rstd_from(self, ps_ap, psB, out_ap, outB, inv_n, tmp_ap, tmpB):
        self.act(tmp_ap, ps_ap, AF.Sqrt, [psB], [tmpB], bias=EPS, scale=inv_n)
        self.recip(out_ap, tmp_ap, [tmpB], [outB])

    def rmsnorm(self, gi, dst_bf=True, cb32=None, cb_sq=None):
        with self.scope() as sc:
            sqp = RPool(sc, "sq", 2, [128, KC, 512], BF16)
            rsp = RPool(sc, "rs", 2, [128, 512], F32)
            tmp = RPool(sc, "rt", 2, [128, 512], F32)
            h32p = RPool(sc, "h32", 2, [128, KC, 512], F32) if cb32 is not None else None
            for tb in range(NB):
                tsl = slice(tb * 512, (tb + 1) * 512)
                sq, sqB = sqp.next()
                hB = [self.hB[c][tb] for c in range(KC)]
                self.act(sq[:], self.hT[:, :, tsl], AF.Square, hB, sqB)
                ps, psB = self.psn()
                for c in range(KC):
                    self.mm(ps[:], self.ones_bf[:], sq[:, c, :], c == 0, c == KC - 1, sqB + [self.cB], [psB])
                if cb_sq is not None:
                    cb_sq(tb, sq, sqB)
                rs, rsB = rsp.next()
                t_, tB = tmp.next()
                self.rstd_from(ps[:], psB, rs[:], rsB[0], 1.0 / D, t_[:], tB[0])
                if dst_bf:
                    for c in range(KC):
                        self.stt(self.hnT[:, c, tsl], self.hT[:, c, tsl], self.sm[:, SM_G + gi * 8 + c:SM_G + gi * 8 + c + 1],
                                 rs[:], ALU.mult, ALU.mult, [self.hB[c][tb], rsB[0], self.cB], [self.hnB[tb]])
                if cb32 is not None:
                    h32, h32B = h32p.next()
                    for c in range(KC):
                        self.stt(h32[:, c, :], self.hT[:, c, tsl], self.sm[:, SM_G + gi * 8 + c:SM_G + gi * 8 + c + 1],
                                 rs[:], ALU.mult, ALU.mult, [self.hB[c][tb], rsB[0], self.cB], h32B)
                    cb32(tb, h32, h32B[0])

    def proj(self, ps, psB, wt, wB, col0, tb, src=None, srcB=None, nk=KC):
        src = self.hnT if src is None else src
        srcB = self.hnB[tb] if srcB is None else srcB
        tsl = slice(tb * 512, (tb + 1) * 512)
        for kc in range(nk):
            self.mm(ps[:], wt[:, kc, col0:col0 + 128], src[:, kc, tsl], kc == 0, kc == nk - 1, [wB, srcB], [psB])

    def outproj(self, w_dram, r0):
        with self.scope() as sc:
            wo = sc.sb("wo", [128, 4, D], BF16)
            woB = Buf("wo")
            self.wload(wo[:], w_dram[r0:r0 + 512, :].rearrange("(j p) n -> p j n", p=128), woB)
            for d in range(KC):
                for tb in range(NB):
                    tsl = slice(tb * 512, (tb + 1) * 512)
                    ps, psB = self.psn()
                    for j in range(4):
                        self.mm(ps[:], wo[:, j, d * 128:(d + 1) * 128], self.yT[:, j, tsl], j == 0, j == 3,
                                [woB, self.yB[j][tb]], [psB])
                    self.tt(self.hT[:, d, tsl], self.hT[:, d, tsl], ps[:], ALU.add, [psB, self.hB[d][tb]], [self.hB[d][tb]])

    def swiglu_multi(self, jobs, pools, after_first_load=None):
        wp, wdp, sgp = pools
        groups = []
        for ji, (wgu, wd, F, gs) in enumerate(jobs):
            nf = F // 128
            for f0 in range(0, nf, 4):
                groups.append((ji, f0, min(4, nf - f0)))

        def load(ji, f0, nfc):
            wgu, wd, F, gs = jobs[ji]
            wg, wgB = wp.next()
            wu, wuB = wp.next()
            wdt, wdB = wdp.next()
            c0 = f0 * 128
            self.wload(wg[:, :, 0:nfc * 128], wgu[:, c0:c0 + nfc * 128].rearrange("(kc p) n -> p kc n", p=128), wgB[0])
            self.wload(wu[:, :, 0:nfc * 128], wgu[:, F + c0:F + c0 + nfc * 128].rearrange("(kc p) n -> p kc n", p=128), wuB[0])
            self.wload(wdt[:, 0:nfc, :], wd[c0:c0 + nfc * 128, :].rearrange("(j p) n -> p j n", p=128), wdB[0])
            return wg, wgB, wu, wuB, wdt, wdB

        nxt = load(*groups[0])
        if after_first_load is not None:
            after_first_load()
        gate = None
        for gi, (ji, f0, nfc) in enumerate(groups):
            wg, wgB, wu, wuB, wdt, wdB = nxt
            nxt = load(*groups[gi + 1]) if gi + 1 < len(groups) else None
            if f0 == 0:
                gs = jobs[ji][3]
                gate = gs() if gs is not None else None
            for j in range(nfc):
                for tb in range(NB):
                    tsl = slice(tb * 512, (tb + 1) * 512)
                    pg, pgB = self.psn()
                    self.proj(pg, pgB, wg, wgB[0], j * 128, tb)
                    pu, puB = self.psn()
                    self.proj(pu, puB, wu, wuB[0], j * 128, tb)
                    sg, sgB = sgp.next()
                    self.act(sg[:], pg[:], AF.Silu, [pgB], sgB)
                    if gate is not None:
                        self.tt(sg[:], sg[:], gate[0][:, tsl], ALU.mult, [sgB[0], gate[1]], sgB)
                    self.tt(self.yT[:, j, tsl], sg[:], pu[:], ALU.mult, [sgB[0], puB], [self.yB[j][tb]])
            for d in range(KC):
                for tb in range(NB):
                    tsl = slice(tb * 512, (tb + 1) * 512)
                    ps, psB = self.psn()
                    for j in range(nfc):
                        self.mm(ps[:], wdt[:, j, d * 128:(d + 1) * 128], self.yT[:, j, tsl], j == 0, j == nfc - 1,
                                [wdB[0], self.yB[j][tb]], [psB])
                    self.tt(self.hT[:, d, tsl], self.hT[:, d, tsl], ps[:], ALU.add, [psB, self.hB[d][tb]], [self.hB[d][tb]])

    def ffn_pools(self, sc):
        return (RPool(sc, "wp", 4, [128, KC, 512], BF16), RPool(sc, "wdp", 2, [128, 4, D], BF16),
                RPool(sc, "sg", 4, [128, 512], F32))

    def load_x(self, b):
        with self.scope() as sc:
            xp = RPool(sc, "xin", 3, [128, D], F32)
            for tt in range(NT):
                xt, xB = xp.next()
                self.P.dma(SP, xt[:], self.x[b, tt * 128:(tt + 1) * 128, :], [], xB)
                for half in range(2):
                    ps, psB = self.psn()
                    for j in range(4):
                        c = half * 4 + j
                        self.tr(ps[:, j * 128:(j + 1) * 128], xt[:, c * 128:(c + 1) * 128], xB, [psB])
                    wB = [self.hB[half * 4 + j][tt // 4] for j in range(4)]
                    eng = ACT if half == 0 else DVE
                    self.cp(eng, self.hT[:, half * 4:half * 4 + 4, tt * 128:(tt + 1) * 128],
                            ps[:].rearrange("p (j t) -> p j t", j=4), [psB], wB)

    def store_out(self, b, final):
        with self.scope() as sc:
            op_ = RPool(sc, "ost", 3, [128, D], F32)

            def emit_block(tb, src, srcB):
                for t4 in range(4):
                    tt = tb * 4 + t4
                    ot, oB = op_.next()
                    for half in range(2):
                        ps, psB = self.psn()
                        for j in range(4):
                            c = half * 4 + j
                            self.tr(ps[:, j * 128:(j + 1) * 128], src(c, t4), srcB, [psB])
                        eng = ACT if half == 0 else DVE
                        self.cp(eng, ot[:, half * 512:(half + 1) * 512], ps[:], [psB], oB)
                    self.P.dma(SP, self.out[b, tt * 128:(tt + 1) * 128, :], ot[:], oB, [self.outB])

            if final:
                self.rmsnorm(6, dst_bf=False,
                             cb32=lambda tb, h32, hB_: emit_block(tb, lambda c, t4: h32[:, c, t4 * 128:(t4 + 1) * 128], [hB_]))
            else:
                for tb in range(NB):
                    emit_block(tb, lambda c, t4, tb=tb: self.hT[:, c, tb * 512 + t4 * 128: tb * 512 + (t4 + 1) * 128],
                               [self.hB[c][tb] for c in range(KC)])

    def mixer0(self):
        w_in = self.w["ev_w_in"]
        self.rmsnorm(0)
        sm = self.sm
        if self.dbg == 1:
            return
        with self.scope() as sc:
            wts = []
            for i in range(3):
                t = sc.sb("wa", [128, KC, 512], BF16)
                tB = Buf("wa")
                self.wload(t[:], w_in[:, i * 512:(i + 1) * 512].rearrange("(kc p) n -> p kc n", p=128), tB)
                wts.append((t, tB))
            bgp = RPool(sc, "bg", 2, [128, S], F32)
            zp = RPool(sc, "z", 2, [128, 2 + S], F32)
            xip = RPool(sc, "xi", 2, [128, 512], F32)
            accp = RPool(sc, "acc", 2, [128, S], F32)
            for t, tB in zip(zp.tiles, zp.bufs):
                self.memset(DVE, t[:, 0:2], 0.0, tB)
            for c in range(4):
                bg, bgB = bgp.next()
                z, zB = zp.next()
                for tb in range(NB):
                    tsl = slice(tb * 512, (tb + 1) * 512)
                    p0, p0B = self.psn()
                    self.proj(p0, p0B, wts[0][0], wts[0][1], c * 128, tb)
                    self.cp(ACT, bg[:, tsl], p0[:], [p0B], bgB)
                    p2, p2B = self.psn()
                    self.proj(p2, p2B, wts[2][0], wts[2][1], c * 128, tb)
                    xi, xiB = xip.next()
                    self.cp(ACT, xi[:], p2[:], [p2B], xiB)
                    p1, p1B = self.psn()
                    self.proj(p1, p1B, wts[1][0], wts[1][1], c * 128, tb)
                    self.tt(z[:, 2 + tb * 512:2 + (tb + 1) * 512], p1[:], xi[:], ALU.mult, [p1B, xiB[0]], zB)
                acc, accB = accp.next()
                cw = lambda j: sm[:, SM_CONVA + c * 3 + j:SM_CONVA + c * 3 + j + 1]
                self.ts(acc[:], z[:, 0:S], cw(0), None, ALU.mult, None, [zB[0], self.cB], accB)
                self.stt(acc[:], z[:, 1:1 + S], cw(1), acc[:], ALU.mult, ALU.add, [zB[0], accB[0], self.cB], accB)
                self.stt(acc[:], z[:, 2:2 + S], cw(2), acc[:], ALU.mult, ALU.add, [zB[0], accB[0], self.cB], accB)
                for tb in range(NB):
                    tsl = slice(tb * 512, (tb + 1) * 512)
                    self.tt(self.yT[:, c, tsl], acc[:, tsl], bg[:, tsl], ALU.mult, [accB[0], bgB[0]], [self.yB[c][tb]])
        if self.dbg == 2:
            return
        self.outproj(self.w["ev_w_out"], 0)
        if self.dbg == 3:
            return
        self.gdn()
        if self.dbg in (4, 5, 6, 7, 8, 71, 72, 73, 74, 75, 711, 712, 713, 714, 76, 77, 78, 79):
            return
        self.outproj(self.w["ev_w_out"], 512)

    def gdn(self):
        w_in = self.w["ev_w_in"]
        sm = self.sm
        cst = self.cst
        cB = self.cB
        SC = 128 ** -0.5
        with self.scope() as sc:
            gw = sc.sb("gw", [128, KC, 8], BF16)
            gwB = Buf()
            self.wload(gw[:], w_in[:, 3584:3592].rearrange("(kc p) n -> p kc n", p=128), gwB)
            graw = sc.sb("graw", [128, NT, 8], F32)
            G = {n: (sc.sb(n, [128, NT, 4], F32), Buf(n)) for n in
                 ["g", "beta", "nbeta", "gc", "gtot", "egc", "eglast", "ekd", "nbeg", "t1"]}
            grB = Buf()
            ps, psB = self.psn()
            for tt in range(NT):
                for kc in range(KC):
                    self.mm(ps[:, tt * 8:(tt + 1) * 8], self.hnT[:, kc, tt * 128:(tt + 1) * 128], gw[:, kc, :],
                            kc == 0, kc == KC - 1, [gwB, self.hnB[tt // 4]], [psB])
            self.cp(DVE, graw[:], ps[:, 0:128].rearrange("p (t e) -> p t e", e=8), [psB], [grB])
            g, gB = G["g"]
            t1, t1B = G["t1"]
            bro = lambda c0: sm[:, c0:c0 + 4].unsqueeze(1).to_broadcast([128, NT, 4])
            self.tt(t1[:], graw[:, :, 0:4], bro(SM_DTB), ALU.add, [grB, cB], [t1B])
            self.act(t1[:], t1[:], AF.Exp, [t1B], [t1B])
            self.act(t1[:], t1[:], AF.Ln, [t1B], [t1B], bias=1.0)
            self.tt(g[:], t1[:], self.negA[:].unsqueeze(1).to_broadcast([128, NT, 4]), ALU.mult, [t1B, cB], [gB])
            beta, betaB = G["beta"]
            nbeta, nbetaB = G["nbeta"]
            self.act(beta[:], graw[:, :, 4:8], AF.Sigmoid, [grB], [betaB])
            self.ts(nbeta[:], beta[:], -1.0, None, ALU.mult, None, [betaB], [nbetaB])
            gc, gcB = G["gc"]
            gtot, gtotB = G["gtot"]
            ps, psB = self.psn()
            gflat = g[:].rearrange("p t e -> p (t e)")
            self.mm(ps[:, 0:64], cst[:, C_U:C_U + 128], gflat, True, True, [gB, cB], [psB])
            self.cp(DVE, gc[:].rearrange("p t e -> p (t e)"), ps[:, 0:64], [psB], [gcB])
            ps, psB = self.psn()
            self.mm(ps[:, 0:64], self.ones32[:], gflat, True, True, [gB, cB], [psB])
            self.cp(DVE, gtot[:].rearrange("p t e -> p (t e)"), ps[:, 0:64], [psB], [gtotB])
            egc, egcB = G["egc"]
            eglast, eglB = G["eglast"]
            ekd, ekdB = G["ekd"]
            nbeg, nbegB = G["nbeg"]
            self.act(egc[:], gc[:], AF.Exp, [gcB], [egcB])
            self.act(eglast[:], gtot[:], AF.Exp, [gtotB], [eglB])
            self.tt(ekd[:], gtot[:], gc[:], ALU.subtract, [gtotB, gcB], [ekdB])
            self.act(ekd[:], ekd[:], AF.Exp, [ekdB], [ekdB])
            self.tt(nbeg[:], nbeta[:], egc[:], ALU.mult, [nbetaB, egcB], [nbegB])

            if self.dbg == 4:
                return
            wtp = RPool(sc, "wh", 1, [128, KC, 512], BF16, nb=4)
            Wa = sc.sb("Wa", [128, 4 + S], BF16)
            WaB = Buf()
            Wb = sc.sb("Wb", [128, S], F32)
            WbB = Buf()
            self.memset(DVE, Wa[:, 0:4], 0.0, [WaB])
            dgc = sc.sb("dgc", [128, 4, 128], BF16)
            dgcB = Buf()
            qT = sc.sb("qT", [128, S], BF16)
            kT = sc.sb("kT", [128, S], BF16)
            vT = sc.sb("vT", [128, S], BF16)
            sqb = sc.sb("sqb", [128, S], BF16)
            qTB, kTB, vTB, sqbB = bufs(NB, "qT"), bufs(NB, "kT"), bufs(NB, "vT"), bufs(NB, "sqb")
            ktok = sc.sb("ktok", [128, NT, 128], BF16)
            vtok = sc.sb("vtok", [128, NT, 128], BF16)
            ktokB, vtokB = bufs(2, "ktok"), bufs(2, "vtok")
            oT = Wb[:, 0:S]
            oTB = [WbB] * NT
            rsp = RPool(sc, "grs", 1, [128, 512], F32)
            rtp = RPool(sc, "grt", 1, [128, 512], F32)
            KU = 3
            slots = []
            for k_ in range(KU):
                slots.append({
                    "f32p": RPool(sc, f"u32_{k_}_", 5, [128, 128], F32),
                    "xyp": RPool(sc, f"xy_{k_}_", 3, [128, 256], BF16),
                    "pp": RPool(sc, f"pp_{k_}_", 3, [128, 128], BF16),
                    "up": {n: RPool(sc, f"{n}_{k_}_", 2, [128, 128], BF16) for n in ["TT", "wTn", "vb", "qkT", "qgT", "kdec", "nkbg"]},
                    "upi": {n: 0 for n in ["TT", "wTn", "vb", "qkT", "qgT", "kdec", "nkbg"]},
                    "banks": [2 * k_, 2 * k_ + 1],
                })
            vnp = RPool(sc, "vnew", 2, [128, 128], BF16)
            S32 = sc.sb("S32", [128, 128], F32)
            Sb = sc.sb("Sb", [128, 128], BF16)
            S32B, SbB = Buf(), Buf()

            def l2n_block(tb, dst, dstB):
                tsl = slice(tb * 512, (tb + 1) * 512)
                self.act(sqb[:, tsl], Wb[:, tsl], AF.Square, [WbB], [sqbB[tb]])
                ps, psB = self.psn()
                self.mm(ps[:], self.ones_bf[:], sqb[:, tsl], True, True, [sqbB[tb], cB], [psB])
                rs, rsB = rsp.next()
                rt, rtB = rtp.next()
                self.rstd_from(ps[:], psB, rs[:], rsB[0], 1.0, rt[:], rtB[0])
                self.tt(dst[:, tsl], Wb[:, tsl], rs[:], ALU.mult, [WbB, rsB[0]], [dstB[tb]])

            def load_head(h):
                wt, wtB = wtp.next()
                for i in range(4):
                    c0 = 1536 + i * 512 + h * 128
                    self.wload(wt[:, :, i * 128:(i + 1) * 128], w_in[:, c0:c0 + 128].rearrange("(kc p) n -> p kc n", p=128), wtB[i])
                return wt, wtB

            nxt_w = load_head(0)
            for h in range(4):
                wt, wtB = nxt_w
                for typ, dst, dstB in ((0, qT, qTB), (1, kT, kTB), (2, vT, vTB)):
                    for tb in range(NB):
                        ps, psB = self.psn()
                        self.proj(ps, psB, wt, wtB[typ], typ * 128, tb)
                        self.cp(ACT, Wa[:, 4 + tb * 512:4 + (tb + 1) * 512], ps[:], [psB], [WaB])
                    ci = typ * 4 + h
                    for j in range(4):
                        self.ts(dgc[:, j, :], self.ident_bf[:], sm[:, SM_GCONV + ci * 4 + j:SM_GCONV + ci * 4 + j + 1], None,
                                ALU.mult, None, [cB, WaB, dgcB], [dgcB])
                    for tb in range(NB):
                        tsl = slice(tb * 512, (tb + 1) * 512)
                        pc_, pcB = self.psn()
                        for j in range(4):
                            self.mm(pc_[:], dgc[:, j, :], Wa[:, 1 + tb * 512 + j:1 + tb * 512 + j + 512], j == 0, j == 3,
                                    [dgcB, WaB], [pcB])
                        if typ == 2:
                            self.act(vT[:, tsl], pc_[:], AF.Silu, [pcB], [vTB[tb]])
                        else:
                            self.act(Wb[:, tsl], pc_[:], AF.Silu, [pcB], [WbB])
                    if typ != 2:
                        for tb in range(NB):
                            l2n_block(tb, dst, dstB)
                if self.dbg == 5:
                    return
                for src, srcB, dst, dstB in ((kT, kTB, ktok, ktokB), (vT, vTB, vtok, vtokB)):
                    for half in range(2):
                        for j in range(8):
                            tt = half * 8 + j
                            self.P.op(PE, lambda e, o=self.psbf[:, j * 128:(j + 1) * 128], i_=src[:, tt * 128:(tt + 1) * 128]:
                                      e.transpose(out=o, in_=i_, identity=self.ident_bf[:]), [srcB[tt // 4], cB], [self.psbfB])
                        self.cp(DVE if half else ACT, dst[:, half * 8:half * 8 + 8, :],
                                self.psbf[:].rearrange("p (t e) -> p t e", e=128), [self.psbfB], [dstB[half]])
                if self.dbg == 6:
                    return
                self.memset(DVE, S32[:], 0.0, [S32B])
                self.memset(DVE, Sb[:], 0.0, [SbB])
                def unit_intra(n, sl, res):
                    tl = slice(n * 128, (n + 1) * 128)
                    tb = n // 4
                    col = lambda a: a[:, n, h:h + 1]
                    f32p, xyp, pp, up = sl["f32p"], sl["xyp"], sl["pp"], sl["up"]
                    for pl_ in [f32p, xyp, pp]:
                        pl_.i = 0
                    for nm in up:
                        up[nm].i = sl["upi"][nm]
                        sl["upi"][nm] ^= 1
                    bk = {"i": 0}

                    def psn_():
                        j = sl["banks"][bk["i"]]
                        bk["i"] = (bk["i"] + 1) % 2
                        return self.ps[j], self.psB[j]
                    dg, dgB = f32p.next()
                    self.ts(dg[:], cst[:, C_ID:C_ID + 128], col(gc), None, ALU.mult, None, [cB, gcB], dgB)
                    pr, prB = psn_()
                    self.mm(pr[:, 0:128], self.ones32[:], dg[:], True, True, [dgB[0], cB], [prB])
                    yield
                    aU, aUB = f32p.next()
                    aL, aLB = f32p.next()
                    eR, eRB = f32p.next()
                    self.cp(ACT, dg[:], pr[:, 0:128], [prB, dgB[0]], dgB)
                    yield
                    self.stt(aL[:], dg[:], col(gc), cst[:, C_POSL:C_POSL + 128], ALU.subtract, ALU.add, [dgB[0], gcB, cB], aLB)
                    self.stt(aU[:], dg[:], col(gc), cst[:, C_NEGU:C_NEGU + 128], ALU.subtract, ALU.add, [dgB[0], gcB, cB], aUB)
                    self.act(eR[:], dg[:], AF.Exp, dgB, eRB)
                    yield
                    self.act(aL[:], aL[:], AF.Exp, aLB, aLB, scale=-1.0)
                    self.act(aU[:], aU[:], AF.Exp, aUB, aUB)
                    pg, pgB = psn_()
                    self.mm(pg[:, 0:128], kT[:, tl], kT[:, tl], True, True, [kTB[tb]], [pgB])
                    yield
                    A32, A32B = f32p.next()
                    self.stt(A32[:], pg[:, 0:128], col(nbeta), aL[:], ALU.mult, ALU.mult, [pgB, nbetaB, aLB[0]], A32B)
                    yield
                    xy, xyB = xyp.next()
                    self.cp(ACT, xy[:, 0:128], A32[:], A32B, xyB)
                    pt, ptB = psn_()
                    self.tr(pt[:, 0:128], A32[:], A32B, [ptB])
                    yield
                    self.cp(ACT, xy[:, 128:256], pt[:, 0:128], [ptB, xyB[0]], xyB)
                    yield
                    Pk, PkB = pp.next()
                    self.tt(Pk[:], xy[:, 128:256], self.ident_bf[:], ALU.add, [xyB[0], cB], PkB)
                    for lv in range(1, 7):
                        px, pxB = psn_()
                        X, Y = xy[:, 0:128], xy[:, 128:256]
                        self.mm(px[:, 0:128], Y, X, True, True, xyB, [pxB])
                        if lv < 6:
                            self.mm(px[:, 128:256], X, Y, True, True, xyB, [pxB])
                        yield
                        xy2, xy2B = xyp.next()
                        wdt = 256 if lv < 6 else 128
                        self.cp(ACT, xy2[:, 0:wdt], px[:, 0:wdt], [pxB], xy2B)
                        yield
                        p2, p2B = psn_()
                        self.mm(p2[:, 0:128], xy2[:, 0:128], Pk[:], True, True, [xy2B[0], PkB[0]], [p2B])
                        yield
                        if lv < 6:
                            Pn, PnB = pp.next()
                        else:
                            Pn, PnB = up["TT"].next()
                        self.tt(Pn[:], Pk[:], p2[:, 0:128], ALU.add, [PkB[0], p2B], PnB)
                        Pk, PkB = Pn, PnB
                        xy, xyB = xy2, xy2B
                    TT, TTB = Pk, PkB
                    nkbg, nkbgB = up["nkbg"].next()
                    vb, vbB = up["vb"].next()
                    kdec, kdecB = up["kdec"].next()
                    self.ts(nkbg[:], ktok[:, n, :], col(nbeg), None, ALU.mult, None, [ktokB[n // 8], nbegB], nkbgB)
                    self.ts(vb[:], vtok[:, n, :], col(beta), None, ALU.mult, None, [vtokB[n // 8], betaB], vbB)
                    self.ts(kdec[:], ktok[:, n, :], col(ekd), None, ALU.mult, None, [ktokB[n // 8], ekdB], kdecB)
                    yield
                    pw, pwB = psn_()
                    self.mm(pw[:, 0:128], nkbg[:], TT[:], True, True, [nkbgB[0], TTB[0]], [pwB])
                    pq, pqB = psn_()
                    self.mm(pq[:, 0:128], kT[:, tl], qT[:, tl], True, True, [kTB[tb], qTB[tb]], [pqB])
                    yield
                    wTn, wTnB = up["wTn"].next()
                    self.cp(ACT, wTn[:], pw[:, 0:128], [pwB], wTnB)
                    qkT, qkTB = up["qkT"].next()
                    self.stt(qkT[:], pq[:, 0:128], SC, aU[:], ALU.mult, ALU.mult, [pqB, aUB[0]], qkTB)
                    qgT, qgTB = up["qgT"].next()
                    self.stt(qgT[:], eR[:], SC, qT[:, tl], ALU.mult, ALU.mult, [eRB[0], qTB[tb]], qgTB)
                    res[n] = dict(TT=(TT, TTB), vb=(vb, vbB), kdec=(kdec, kdecB), wTn=(wTn, wTnB), qkT=(qkT, qkTB), qgT=(qgT, qgTB))
                    yield

                def recur(ns, res):
                    b6, b6B = self.ps[6], self.psB[6]
                    for n in ns:
                        tl = slice(n * 128, (n + 1) * 128)
                        col = lambda a: a[:, n, h:h + 1]
                        r = res[n]
                        TT, TTB = r["TT"]
                        vb, vbB = r["vb"]
                        kdec, kdecB = r["kdec"]
                        wTn, wTnB = r["wTn"]
                        qkT, qkTB = r["qkT"]
                        qgT, qgTB = r["qgT"]
                        self.mm(b6[:, 0:128], TT[:], vb[:], True, False, [TTB[0], vbB[0]], [b6B])
                        self.mm(b6[:, 0:128], wTn[:], Sb[:], False, True, [wTnB[0], SbB], [b6B])
                        yield
                        vnew, vnewB = vnp.next()
                        self.cp(ACT, vnew[:], b6[:, 0:128], [b6B], vnewB)
                        yield
                        self.mm(b6[:, 128:256], Sb[:], qgT[:], True, False, [SbB, qgTB[0]], [b6B])
                        self.mm(b6[:, 128:256], vnew[:], qkT[:], False, True, [vnewB[0], qkTB[0]], [b6B])
                        self.mm(b6[:, 256:384], kdec[:], vnew[:], True, True, [kdecB[0], vnewB[0]], [b6B])
                        yield
                        self.cp(DVE, oT[:, tl], b6[:, 128:256], [b6B], [oTB[n]])
                        self.stt(S32[:], S32[:], col(eglast), b6[:, 256:384], ALU.mult, ALU.add, [S32B, eglB, b6B], [S32B])
                        yield
                        self.cp(ACT, Sb[:], S32[:], [S32B], [SbB])
                        yield

                def drive(gens):
                    gens = list(gens)
                    while gens:
                        nxt_ = []
                        for g_ in gens:
                            try:
                                next(g_)
                                nxt_.append(g_)
                            except StopIteration:
                                pass
                        gens = nxt_

                res = {}
                prev_rec = None
                for n0 in range(0, NT, KU):
                    ns = list(range(n0, min(n0 + KU, NT)))
                    gens = [unit_intra(n, slots[i_], res) for i_, n in enumerate(ns)]
                    if prev_rec is not None:
                        gens.append(prev_rec)
                    drive(gens)
                    prev_rec = recur(ns, res)
                drive([prev_rec])
                for tb in range(NB):
                    tsl = slice(tb * 512, (tb + 1) * 512)
                    oB_ = [WbB]
                    self.act(sqb[:, tsl], oT[:, tsl], AF.Square, oB_, [sqbB[tb]])
                    ps, psB = self.psn()
                    self.mm(ps[:], self.ones_bf[:], sqb[:, tsl], True, True, [sqbB[tb], cB], [psB])
                    rs, rsB = rsp.next()
                    rt, rtB = rtp.next()
                    self.rstd_from(ps[:], psB, rs[:], rsB[0], 1.0 / 128, rt[:], rtB[0])
                    pg, pgB = self.psn()
                    self.proj(pg, pgB, wt, wtB[3], 3 * 128, tb)
                    self.act(rt[:], pg[:], AF.Silu, [pgB], rtB)
                    self.stt(rs[:], oT[:, tsl], sm[:, SM_GDNG:SM_GDNG + 1], rs[:], ALU.mult, ALU.mult, oB_ + [rsB[0], cB], rsB)
                    self.tt(self.yT[:, h, tsl], rs[:], rt[:], ALU.mult, [rsB[0], rtB[0]], [self.yB[h][tb]])
                if h < 3:
                    nxt_w = load_head(h + 1)

    def ffn0(self):
        self.rmsnorm(1)
        with self.scope() as sc:
            self.swiglu_multi([(self.w["ffn_w_gate_up"], self.w["ffn_w_down"], D_FF, None)], self.ffn_pools(sc))

    def ple(self, b, i):
        self.rmsnorm(2 + 3 * i)
        with self.scope() as sc:
            pT = sc.sb("pT", [128, 2, S], BF16)
            pTB = bufs(NB, "pT")
            pin = RPool(sc, "pin", 3, [128, 256], F32)
            wg0 = sc.sb("wg0", [128, KC, 512], BF16)
            wg1 = sc.sb("wg1", [128, KC, 512], BF16)
            wpj = sc.sb("wpj", [128, 2, D], BF16)
            wg0B, wg1B, wpjB = Buf(), Buf(), Buf()
            wgd = self.w["ple_w_gate"][i]
            self.wload(wg0[:], wgd[:, 0:512].rearrange("(kc p) n -> p kc n", p=128), wg0B)
            self.wload(wg1[:], wgd[:, 512:1024].rearrange("(kc p) n -> p kc n", p=128), wg1B)
            self.wload(wpj[:], self.w["ple_w_proj"][i].rearrange("(kc p) n -> p kc n", p=128), wpjB)
            for tt in range(NT):
                pt_, ptB = pin.next()
                self.P.dma(SP, pt_[:], self.p[i, b, tt * 128:(tt + 1) * 128, :], [], ptB)
                ps, psB = self.psn()
                for j in range(2):
                    self.tr(ps[:, j * 128:(j + 1) * 128], pt_[:, j * 128:(j + 1) * 128], ptB, [psB])
                self.cp(ACT, pT[:, :, tt * 128:(tt + 1) * 128], ps[:, 0:256].rearrange("p (j t) -> p j t", j=2), [psB], [pTB[tt // 4]])
            sgp = RPool(sc, "psg", 3, [128, 512], F32)
            for d in range(KC):
                wg, wgB = (wg0, wg0B) if d < 4 else (wg1, wg1B)
                for tb in range(NB):
                    tsl = slice(tb * 512, (tb + 1) * 512)
                    pg, pgB = self.psn()
                    self.proj(pg, pgB, wg, wgB, (d % 4) * 128, tb)
                    pp_, ppB = self.psn()
                    for kc in range(2):
                        self.mm(pp_[:], wpj[:, kc, d * 128:(d + 1) * 128], pT[:, kc, tsl], kc == 0, kc == 1, [wpjB, pTB[tb]], [ppB])
                    sg, sgB = sgp.next()
                    self.act(sg[:], pg[:], AF.Sigmoid, [pgB], sgB)
                    self.tt(sg[:], sg[:], pp_[:], ALU.mult, [sgB[0], ppB], sgB)
                    self.tt(self.hT[:, d, tsl], self.hT[:, d, tsl], sg[:], ALU.add, [sgB[0], self.hB[d][tb]], [self.hB[d][tb]], eng=POOL)

    def mixer1(self):
        w_in = self.w["od_w_in"]
        sm = self.sm
        cB = self.cB
        self.rmsnorm(3)
        with self.scope() as sc:
            qT = sc.sb("aq", [128, 4, S], BF16)
            kT = sc.sb("ak", [128, 4, S], BF16)
            qB = [bufs(NB, "aq") for _ in range(4)]
            kB = [bufs(NB, "ak") for _ in range(4)]
            vtok = sc.sb("av", [128, NT, 512], BF16)
            vB = bufs(NT, "av")
            wtp = RPool(sc, "aw", 2, [128, KC, 512], BF16)
            for typ, dst, dstB, scl in ((0, qT, qB, 0.125), (1, kT, kB, 1.0)):
                wt, wtB = wtp.next()
                self.wload(wt[:], w_in[:, typ * 512:(typ + 1) * 512].rearrange("(kc p) n -> p kc n", p=128), wtB[0])
                for h in range(4):
                    for tb in range(NB):
                        ps, psB = self.psn()
                        self.proj(ps, psB, wt, wtB[0], h * 128, tb)
                        self.act(dst[:, h, tb * 512:(tb + 1) * 512], ps[:], AF.Copy, [psB], [dstB[h][tb]], scale=scl)
            wt, wtB = wtp.next()
            self.wload(wt[:], w_in[:, 1024:1536].rearrange("(kc p) n -> p kc n", p=128), wtB[0])
            for tt in range(NT):
                ps, psB = self.psn()
                for kc in range(KC):
                    self.mm(ps[:], self.hnT[:, kc, tt * 128:(tt + 1) * 128], wt[:, kc, :], kc == 0, kc == KC - 1,
                            [self.hnB[tt // 4], wtB[0]], [psB])
                self.cp(DVE if tt % 2 else ACT, vtok[:, tt, :], ps[:], [psB], [vB[tt]])
            ptp = RPool(sc, "pT", 6, [128, 512], BF16)
            tmp = RPool(sc, "atmp", 3, [128, 256], F32)
            rcp = RPool(sc, "arc", 4, [128, 512], F32)
            sqp = RPool(sc, "asq", 2, [128, 512], BF16)
            NS_ = 3
            LA = 2
            st_ = {"si": 0}

            def next_s():
                i = st_["si"]
                st_["si"] = (i + 1) % NS_
                return self.ps[i], self.psB[i]

            tiles = []
            for h in range(4):
                for qc in range(NB):
                    nkt = 4 * qc + 4
                    for kt in range(nkt):
                        for m in range(2):
                            tiles.append((h, qc, kt, m, nkt))

            def stageA(h, qc, kt, m, nkt):
                c31 = sm[:, SM_TBL + 31 * 4 + h:SM_TBL + 31 * 4 + h + 1]
                d = kt - 4 * qc
                qlo = max(0, d) * 128
                pss, pssB = next_s()
                prt = slice(m * 64, (m + 1) * 64)
                self.mm(pss[:, qlo:512], kT[prt, h, kt * 128:(kt + 1) * 128], qT[prt, h, qc * 512 + qlo:(qc + 1) * 512],
                        True, True, [kB[h][kt // 4], qB[h][qc]], [pssB])
                pT_, pTB = ptp.next()
                if d >= 0:
                    n0, n1, b0 = qlo, min(qlo + 256, 512), 0
                elif d == -1:
                    n0, n1, b0 = 0, 128, 128
                else:
                    n0 = n1 = b0 = 0
                if n1 > n0:
                    t_, tB = tmp.next()
                    w_ = n1 - n0
                    self.tt(t_[:, 0:w_], pss[:, n0:n1], self.Bh[:, h, b0:b0 + w_], ALU.add, [pssB, cB], tB)
                    self.act(pT_[:, n0:n1], t_[:, 0:w_], AF.Exp, tB, pTB)
                if n1 < 512:
                    f0 = max(n1, qlo)
                    self.act(pT_[:, f0:512], pss[:, f0:512], AF.Exp, [pssB, cB], pTB, bias=c31)
                return pT_, pTB, qlo

            def stageB(tile, info):
                h, qc, kt, m, nkt = tile
                pT_, pTB, qlo = info
                self.mm(self.ps[3 + m][:, qlo:512], vtok[:, kt, h * 128:(h + 1) * 128], pT_[:, qlo:512],
                        kt == 0, kt == nkt - 1, [vB[kt], pTB[0]], [self.psB[3 + m]])
                self.mm(self.ps[5 + m][:, qlo:512], self.ones_bf[:], pT_[:, qlo:512],
                        kt == 0, kt == nkt - 1, [cB, pTB[0]], [self.psB[5 + m]])
                if kt == nkt - 1 and m == 1:
                    r1, r1B = rcp.next()
                    r2, r2B = rcp.next()
                    self.recip(r1[:], self.ps[5][:], [self.psB[5]], r1B)
                    self.recip(r2[:], self.ps[6][:], [self.psB[6]], r2B)
                    self.tt(r1[:], r1[:], self.ps[3][:], ALU.mult, [r1B[0], self.psB[3]], r1B)
                    self.stt(r2[:], r2[:], self.neglam[:, 0:1], self.ps[4][:], ALU.mult, ALU.mult, [r2B[0], self.psB[4], cB], r2B)
                    self.tt(r1[:], r1[:], r2[:], ALU.add, [r1B[0], r2B[0]], r1B)
                    sq, sqB = sqp.next()
                    self.act(sq[:], r1[:], AF.Square, r1B, sqB)
                    pn, pnB = next_s()
                    self.mm(pn[:], self.ones_bf[:], sq[:], True, True, sqB + [cB], [pnB])
                    self.rstd_from(pn[:], pnB, r2[:], r2B[0], 1.0 / 128, r2[:], r2B[0])
                    self.stt(self.yT[:, h, qc * 512:(qc + 1) * 512], r1[:], self.dgl[:, 0:1], r2[:], ALU.mult, ALU.mult,
                             [r1B[0], r2B[0], cB], [self.yB[h][qc]])

            infos = {}
            for i in range(min(LA, len(tiles))):
                infos[i] = stageA(*tiles[i])
            for i in range(len(tiles)):
                if i + LA < len(tiles):
                    infos[i + LA] = stageA(*tiles[i + LA])
                stageB(tiles[i], infos.pop(i))
        self.outproj(self.w["od_w_out"], 0)
        with self.scope() as sc:
            wa = sc.sb("cwa", [128, KC, 512], BF16)
            wb = sc.sb("cwb", [128, KC, 512], BF16)
            waB, wbB = Buf(), Buf()
            self.wload(wa[:], w_in[:, 1536:2048].rearrange("(kc p) n -> p kc n", p=128), waB)
            self.wload(wb[:], w_in[:, 2048:2560].rearrange("(kc p) n -> p kc n", p=128), wbB)
            Wc = RPool(sc, "Wc", 1, [128, 32 + S], BF16)
            for t, tB in zip(Wc.tiles, Wc.bufs):
                self.memset(DVE, t[:, 0:32], 0.0, tB)
            cc = sc.sb("cc", [128, 4, S], F32)
            ccB = [bufs(NB, "cc") for _ in range(4)]
            dgp = RPool(sc, "dgm", 1, [128, 31, 128], BF16)
            sgp = RPool(sc, "csg", 2, [128, 512], F32)
            for c in range(4):
                wc, wcB = Wc.next()
                for tb in range(NB):
                    pa, paB = self.psn()
                    self.proj(pa, paB, wa, waB, c * 128, tb)
                    pb, pbB = self.psn()
                    self.proj(pb, pbB, wb, wbB, c * 128, tb)
                    sg, sgB = sgp.next()
                    self.act(sg[:], pb[:], AF.Sigmoid, [pbB], sgB)
                    self.tt(wc[:, 32 + tb * 512:32 + (tb + 1) * 512], pa[:], sg[:], ALU.mult, [paB, sgB[0]], wcB)
                dg, dgB = dgp.next()
                for j in range(31):
                    self.ts(dg[:, j, :], self.ident_bf[:], sm[:, SM_DWW + c * 31 + j:SM_DWW + c * 31 + j + 1], None, ALU.mult, None,
                            [cB, wcB[0]], dgB)
                for tb in range(NB):
                    pc_, pcB = self.psn()
                    for j in range(31):
                        self.mm(pc_[:], dg[:, j, :], wc[:, 2 + tb * 512 + j:2 + tb * 512 + j + 512], j == 0, j == 30,
                                [dgB[0], wcB[0]], [pcB])
                    self.act(cc[:, c, tb * 512:(tb + 1) * 512], pc_[:], AF.Identity, [pcB, cB], [ccB[c][tb]],
                             bias=sm[:, SM_DWB + c:SM_DWB + c + 1])
            rsp = RPool(sc, "crs", 1, [128, 512], F32)
            rtp = RPool(sc, "crt", 1, [128, 512], F32)
            sqp = RPool(sc, "csq", 1, [128, 4, 512], BF16)
            for tb in range(NB):
                tsl = slice(tb * 512, (tb + 1) * 512)
                cBs = [ccB[c][tb] for c in range(4)]
                pm, pmB = self.psn()
                for c in range(4):
                    self.mm(pm[:], self.ones32[:], cc[:, c, tsl], c == 0, c == 3, [ccB[c][tb], cB], [pmB])
                rt, rtB = rtp.next()
                self.ts(rt[:], pm[:], 1.0 / 512, None, ALU.mult, None, [pmB], rtB)
                for c in range(4):
                    self.tt(cc[:, c, tsl], cc[:, c, tsl], rt[:], ALU.subtract, [ccB[c][tb], rtB[0]], [ccB[c][tb]])
                sq, sqB = sqp.next()
                self.act(sq[:], cc[:, :, tsl], AF.Square, cBs, sqB)
                pv, pvB = self.psn()
                for c in range(4):
                    self.mm(pv[:], self.ones_bf[:], sq[:, c, :], c == 0, c == 3, sqB + [cB], [pvB])
                rs, rsB = rsp.next()
                self.rstd_from(pv[:], pvB, rs[:], rsB[0], 1.0 / 512, rt[:], rtB[0])
                for c in range(4):
                    self.tt(cc[:, c, tsl], cc[:, c, tsl], rs[:], ALU.mult, [ccB[c][tb], rsB[0]], [ccB[c][tb]])
                    self.act(self.yT[:, c, tsl], cc[:, c, tsl], AF.Silu, [ccB[c][tb], cB], [self.yB[c][tb]],
                             bias=sm[:, SM_LNB + c:SM_LNB + c + 1], scale=sm[:, SM_LNG + c:SM_LNG + c + 1])
        self.outproj(self.w["od_w_out"], 512)

    def moe(self):
        sm = self.sm
        cB = self.cB
        with self.scope() as sc0:
            lg = sc0.sb("lg", [128, NT, 8], F32)
            lgB = Buf()
            GT = sc0.sb("GT", [8, S], F32)
            GTB = Buf()
            gate = sc0.sb("gate", [128, S], F32)
            gateB = Buf()
            sel = sc0.sb("sel", [8, NEXP * 128], F32)
            selB = Buf()
            self.P.dma(SP, sel[:], self.w["sel"][:, :], [], [selB])

            ss = sc0.sb("mss", [128, NT], F32)
            T = {n: sc0.sb(n, [128, NT, 8], F32) for n in ["eq1", "lg2", "eq2", "Gd"]}
            V = {n: sc0.sb(n, [128, NT], F32) for n in ["m1", "m2", "g1", "g2"]}
            GTBs = bufs(NB, "GT")

            def hook(tb, sq, sqB):
                sl = slice(tb * 4, (tb + 1) * 4)
                B_ = {n: Buf(n) for n in ["ss", "lg", "eq1", "lg2", "eq2", "Gd", "m1", "m2", "g1", "g2"]}
                ps, psB = self.psn()
                for t4 in range(4):
                    for c in range(KC):
                        self.mm(ps[:, t4:t4 + 1], sq[:, c, t4 * 128:(t4 + 1) * 128], self.ones_bf[:, 0:1], c == 0, c == KC - 1,
                                sqB + [cB], [psB])
                for t4 in range(4):
                    tt = tb * 4 + t4
                    for kc in range(KC):
                        self.mm(ps[:, 8 + t4 * 8:16 + t4 * 8], self.hT[:, kc, tt * 128:(tt + 1) * 128], self.gwr[:, kc, :],
                                kc == 0, kc == KC - 1, [self.hB[kc][tb], cB], [psB])
                self.act(ss[:, sl], ps[:, 0:4], AF.Sqrt, [psB], [B_["ss"]], bias=EPS, scale=1.0 / D)
                self.recip(ss[:, sl], ss[:, sl], [B_["ss"]], [B_["ss"]])
                bc = lambda a: a[:, sl].unsqueeze(2).to_broadcast([128, 4, 8])
                lgs = lg[:, sl, :]
                self.tt(lgs, ps[:, 8:40].rearrange("p (t e) -> p t e", e=8), bc(ss), ALU.mult, [psB, B_["ss"]], [B_["lg"]])
                m1, m2, g1, g2 = V["m1"], V["m2"], V["g1"], V["g2"]
                eq1, lg2, eq2, Gd = T["eq1"][:, sl, :], T["lg2"][:, sl, :], T["eq2"][:, sl, :], T["Gd"][:, sl, :]
                self.P.op(DVE, lambda e: e.tensor_reduce(out=m1[:, sl], in_=lgs, axis=AX.X, op=ALU.max), [B_["lg"]], [B_["m1"]])
                self.tt(eq1, lgs, bc(m1), ALU.is_equal, [B_["lg"], B_["m1"]], [B_["eq1"]])
                self.stt(lg2, eq1, -1e9, lgs, ALU.mult, ALU.add, [B_["eq1"], B_["lg"]], [B_["lg2"]])
                self.P.op(DVE, lambda e: e.tensor_reduce(out=m2[:, sl], in_=lg2, axis=AX.X, op=ALU.max), [B_["lg2"]], [B_["m2"]])
                self.tt(eq2, lg2, bc(m2), ALU.is_equal, [B_["lg2"], B_["m2"]], [B_["eq2"]])
                self.tt(g1[:, sl], m1[:, sl], m2[:, sl], ALU.subtract, [B_["m1"], B_["m2"]], [B_["g1"]])
                self.act(g1[:, sl], g1[:, sl], AF.Sigmoid, [B_["g1"]], [B_["g1"]])
                self.ts(g2[:, sl], g1[:, sl], -1.0, 1.0, ALU.mult, ALU.add, [B_["g1"]], [B_["g2"]])
                self.tt(eq1, eq1, bc(g1), ALU.mult, [B_["eq1"], B_["g1"]], [B_["eq1"]])
                self.tt(eq2, eq2, bc(g2), ALU.mult, [B_["eq2"], B_["g2"]], [B_["eq2"]])
                self.tt(Gd, eq1, eq2, ALU.add, [B_["eq1"], B_["eq2"]], [B_["Gd"]])
                pt_, ptB = self.psn()
                for j in range(4):
                    self.tr(pt_[0:8, j * 128:(j + 1) * 128], T["Gd"][:, tb * 4 + j, :], [B_["Gd"]], [ptB])
                self.cp(ACT, GT[:, tb * 512:(tb + 1) * 512], pt_[0:8, :], [ptB], [GTBs[tb]])

            self.rmsnorm(4, cb_sq=hook)
            with self.scope() as sc:
                pools = self.ffn_pools(sc)

                def mk_gate(ex):
                    def f():
                        for tb in range(NB):
                            tsl = slice(tb * 512, (tb + 1) * 512)
                            ps, psB = self.psn()
                            self.mm(ps[:], sel[:, ex * 128:(ex + 1) * 128], GT[:, tsl], True, True, [selB, GTBs[tb]], [psB])
                            self.cp(ACT, gate[:, tsl], ps[:], [psB], [gateB])
                        return gate, gateB
                    return f
                jobs = [(self.w["moe_w_gate_up"][ex], self.w["moe_w_down"][ex], D_FFE, mk_gate(ex)) for ex in range(NEXP)]
                self.swiglu_multi(jobs, pools)

    def setup(self, es, dr):
        nc = self.nc
        sb = lambda n, s, d: es.enter_context(nc.sbuf_tensor("sb_" + n, s, d))
        self.cB = Buf("const")
        cB = self.cB
        self.hT = sb("hT", [128, KC, S], F32)
        self.hnT = sb("hnT", [128, KC, S], BF16)
        self.yT = sb("yT", [128, 4, S], BF16)
        self.hB = [bufs(NB, f"h{c}_") for c in range(KC)]
        self.hnB = bufs(NB, "hn")
        self.yB = [bufs(NB, f"y{c}_") for c in range(4)]
        self.sm = sb("sm", [128, SM_N], F32)
        self.cst = sb("cst", [128, C_N], F32)
        self.ones_bf = sb("ones_bf", [128, 128], BF16)
        self.ident_bf = sb("ident_bf", [128, 128], BF16)
        self.ones32 = sb("ones32", [128, 128], F32)
        self.Bh = sb("Bh", [128, 4, 256], F32)
        self.negA = sb("negA", [128, 4], F32)
        self.gwr = sb("gwr", [128, KC, 8], F32)
        self.neglam = sb("neglam", [128, 1], F32)
        self.dgl = sb("dgl", [128, 1], F32)
        self.ident = self.cst[:, C_ID:C_ID + 128]
        self.ps = [es.enter_context(nc.psum_tensor(f"ps{i}", [128, 512], F32)) for i in range(7)]
        self.psB = bufs(7, "ps")
        self.psbf = es.enter_context(nc.psum_tensor("psbf", [128, 1024], BF16))
        self.psbfB = Buf("psbf")
        self.ps_rot = list(range(7))
        self.ps_i = 0
        self.x, self.p, self.out = dr["x"], dr["p"], dr["out"]
        self.outB = Buf("out")
        self.w = dr
        P = self.P
        P.dma(SP, self.sm[:], dr["small"][:, :], [], [cB])
        P.dma(SP, self.cst[:], dr["cst"][:, :], [], [cB])
        self.memset(DVE, self.ones_bf[:], 1.0, [cB])
        self.memset(DVE, self.ones32[:], 1.0, [cB])
        self.cp(DVE, self.ident_bf[:], self.cst[:, C_ID:C_ID + 128], [cB], [cB])
        sm = self.sm
        for kc in range(KC):
            self.ts(self.gwr[:, kc, :], sm[:, SM_RT + kc * 8:SM_RT + kc * 8 + 8], sm[:, SM_G + 4 * 8 + kc:SM_G + 4 * 8 + kc + 1],
                    None, ALU.mult, None, [cB], [cB])
        self.act(self.negA[:], sm[:, SM_ALOG:SM_ALOG + 4], AF.Exp, [cB], [cB])
        self.ts(self.negA[:], self.negA[:], -1.0, None, ALU.mult, None, [cB], [cB])
        with self.scope() as sc:
            pr = sc.sb("lpr", [128, 128], F32)
            s12 = sc.sb("ls", [128, 2], F32)
            tB = Buf()
            self.tt(pr[:, 0:64], sm[:, SM_LAM:SM_LAM + 64], sm[:, SM_LAM + 64:SM_LAM + 128], ALU.mult, [cB], [tB])
            self.tt(pr[:, 64:128], sm[:, SM_LAM + 128:SM_LAM + 192], sm[:, SM_LAM + 192:SM_LAM + 256], ALU.mult, [cB, tB], [tB])
            P.op(DVE, lambda e: e.tensor_reduce(out=s12[:], in_=pr[:].rearrange("p (a b) -> p a b", a=2), axis=AX.X, op=ALU.add), [tB], [tB])
            self.act(s12[:], s12[:], AF.Exp, [tB], [tB])
            self.tt(self.neglam[:], s12[:, 1:2], s12[:, 0:1], ALU.subtract, [tB], [cB])
            self.ts(self.neglam[:], self.neglam[:], -LAMBDA_INIT, None, ALU.add, None, [cB], [cB])
            self.ts(self.dgl[:], sm[:, SM_DIFFG:SM_DIFFG + 1], 1.0 - LAMBDA_INIT, None, ALU.mult, None, [cB], [cB])
            bm = sc.sb("bm", [128, 32, 256], F32)
            P.dma(SP, bm[:], dr["bmask"][:, :].rearrange("p (b j) -> p b j", b=32), [], [tB])
            for h in range(4):
                self.cp(DVE, self.Bh[:, h, :], self.cst[:, C_NEGM:C_NEGM + 256], [cB], [cB])
                for b in range(32):
                    self.stt(self.Bh[:, h, :], bm[:, b, :], sm[:, SM_TBL + b * 4 + h:SM_TBL + b * 4 + h + 1], self.Bh[:, h, :],
                             ALU.mult, ALU.add, [tB, cB], [cB])

    def sequence(self, b):
        st = self.stop_after
        self.load_x(b)
        stages = [self.mixer0, self.ffn0, lambda: self.ple(b, 0), self.mixer1, self.moe, lambda: self.ple(b, 1)]
        import os
        only = os.environ.get("KONLY")
        for i, f in enumerate(stages):
            if st is not None and i >= st:
                break
            if only is not None and str(i + 1) not in only.split(","):
                continue
            f()
        self.store_out(b, final=(st is None or st >= 7))


W_NAMES = ["ev_w_in", "ev_w_out", "od_w_in", "od_w_out", "ffn_w_gate_up", "ffn_w_down",
           "moe_w_gate_up", "moe_w_down", "ple_w_proj", "ple_w_gate"]
W_SHAPES = {"ev_w_in": [D, EV_IN], "ev_w_out": [D, D], "od_w_in": [D, OD_IN], "od_w_out": [D, D],
            "ffn_w_gate_up": [D, 2 * D_FF], "ffn_w_down": [D_FF, D],
            "moe_w_gate_up": [NEXP, D, 2 * D_FFE], "moe_w_down": [NEXP, D_FFE, D],
            "ple_w_proj": [2, 256, D], "ple_w_gate": [2, D, D]}


def build(nseq=SEQ_PER_CORE, stop_after=None):
    nc = bass.Bass("TRN2", target_bir_lowering=False)
    dr = {}
    dr["x"] = nc.dram_tensor("x", [nseq, S, D], F32, kind="ExternalInput").ap()
    dr["p"] = nc.dram_tensor("p", [2, nseq, S, 256], F32, kind="ExternalInput").ap()
    dr["small"] = nc.dram_tensor("small", [128, SM_N], F32, kind="ExternalInput").ap()
    dr["cst"] = nc.dram_tensor("cst", [128, C_N], F32, kind="ExternalInput").ap()
    dr["sel"] = nc.dram_tensor("sel", [8, NEXP * 128], F32, kind="ExternalInput").ap()
    dr["bmask"] = nc.dram_tensor("bmask", [128, 32 * 256], F32, kind="ExternalInput").ap()
    for n in W_NAMES:
        dr[n] = nc.dram_tensor(n, W_SHAPES[n], F32, kind="ExternalInput").ap()
    dr["out"] = nc.dram_tensor("out", [nseq, S, D], F32, kind="ExternalOutput").ap()
    k = K(nc, nseq, stop_after)
    with ExitStack() as es:
        sems = {e: es.enter_context(nc.semaphore("s_" + e)) for e in ENGS}
        dsems = [es.enter_context(nc.semaphore(f"d{i}")) for i in range(Prog.NDMA)]
        k.setup(es, dr)
        for b in range(nseq):
            k.sequence(b)
        k.P.barrier()
        block = es.enter_context(nc.Block())
        k.P.emit(block, sems, dsems)
    return nc


def _rel_bucket(n):
    n = np.maximum(n, 0)
    nf = np.maximum(n, 1).astype(np.float32)
    large = 16 + (np.log(nf / np.float32(16)) / np.float32(math.log(128 / 16)) * np.float32(16)).astype(np.int32)
    large = np.minimum(large, 31)
    return np.where(n < 16, n, large)


def host_consts():
    i = np.arange(128)[:, None]
    j = np.arange(128)[None, :]
    cst = np.zeros((128, C_N), np.float32)
    cst[:, C_ID:C_ID + 128] = np.eye(128, dtype=np.float32)
    cst[:, C_U:C_U + 128] = (i <= j)
    cst[:, C_NEGU:C_NEGU + 128] = np.where(j >= i, 0.0, NEGBIG)
    cst[:, C_POSL:C_POSL + 128] = np.where(j < i, 0.0, -NEGBIG)
    j2 = np.arange(256)[None, :]
    cst[:, C_NEGM:C_NEGM + 256] = np.where(j2 >= i, 0.0, NEGBIG)
    sel = np.zeros((8, NEXP, 128), np.float32)
    for e in range(NEXP):
        sel[e, e, :] = 1.0
    rel = j2 - i
    bk = _rel_bucket(rel)
    bm = np.zeros((128, 32, 256), np.float32)
    for b in range(32):
        bm[:, b, :] = (bk == b) & (rel >= 0)
    return cst, sel.reshape(8, NEXP * 128), bm.reshape(128, 32 * 256)


def pack_small(inp):
    sm = np.zeros((128, SM_N), np.float32)
    gains = [inp["norm_mix_g"][0], inp["norm_ffn_g"][0], inp["norm_ple_g"][0],
             inp["norm_mix_g"][1], inp["norm_ffn_g"][1], inp["norm_ple_g"][1], inp["final_norm_g"]]
    for gi, g in enumerate(gains):
        sm[:, SM_G + gi * 8:SM_G + gi * 8 + 8] = np.asarray(g).reshape(8, 128).T
    sm[:, SM_CONVA:SM_CONVA + 12] = inp["ev_conv_a"][0].reshape(3, 4, 128).transpose(2, 1, 0).reshape(128, 12)
    sm[:, SM_GCONV:SM_GCONV + 48] = inp["ev_gdn_conv"][0].reshape(4, 12, 128).transpose(2, 1, 0).reshape(128, 48)
    sm[:, SM_GDNG] = inp["ev_gdn_norm_g"][0]
    sm[:, SM_DIFFG] = inp["od_diff_norm_g"][0]
    sm[:, SM_DWW:SM_DWW + 124] = inp["od_conf_dw_w"][0].reshape(31, 4, 128).transpose(2, 1, 0).reshape(128, 124)
    sm[:, SM_DWB:SM_DWB + 4] = inp["od_conf_dw_b"][0].reshape(4, 128).T
    sm[:, SM_LNG:SM_LNG + 4] = inp["od_conf_ln_g"][0].reshape(4, 128).T
    sm[:, SM_LNB:SM_LNB + 4] = inp["od_conf_ln_b"][0].reshape(4, 128).T
    sm[:, SM_RT:SM_RT + 64] = inp["moe_router"][0].reshape(8, 128, 8).transpose(1, 0, 2).reshape(128, 64)
    sm[:, SM_ALOG:SM_ALOG + 4] = np.broadcast_to(inp["ev_gdn_A_log"][0], (128, 4))
    sm[:, SM_DTB:SM_DTB + 4] = np.broadcast_to(inp["ev_gdn_dt_bias"][0], (128, 4))
    sm[:, SM_LAM:SM_LAM + 256] = np.broadcast_to(inp["od_lambda"][0].reshape(256), (128, 256))
    sm[:, SM_TBL:SM_TBL + 128] = np.broadcast_to(inp["rel_bias"].reshape(128), (128, 128))
    return sm


def make_in_maps(inp, nseq, ncores, b0=0):
    cst, sel, bm = host_consts()
    sm = pack_small(inp)
    shared = {"small": sm, "cst": cst, "sel": sel, "bmask": bm,
              "ev_w_in": np.ascontiguousarray(inp["ev_w_in"][0]), "ev_w_out": np.ascontiguousarray(inp["ev_w_out"][0]),
              "od_w_in": np.ascontiguousarray(inp["od_w_in"][0]), "od_w_out": np.ascontiguousarray(inp["od_w_out"][0]),
              "ffn_w_gate_up": np.ascontiguousarray(inp["ffn_w_gate_up"][0]), "ffn_w_down": np.ascontiguousarray(inp["ffn_w_down"][0]),
              "moe_w_gate_up": np.ascontiguousarray(inp["moe_w_gate_up"][0]), "moe_w_down": np.ascontiguousarray(inp["moe_w_down"][0]),
              "ple_w_proj": np.ascontiguousarray(inp["ple_w_proj"]), "ple_w_gate": np.ascontiguousarray(inp["ple_w_gate"])}
    maps = []
    for c in range(ncores):
        m = dict(shared)
        lo = b0 + c * nseq
        m["x"] = np.ascontiguousarray(inp["x"][lo:lo + nseq])
        m["p"] = np.ascontiguousarray(inp["p"][:, lo:lo + nseq])
        maps.append(m)
    return maps


def kernel(**inputs):
    inp = {k: np.asarray(v) for k, v in inputs.items()}
    nc = build(SEQ_PER_CORE, None)
    maps = make_in_maps(inp, SEQ_PER_CORE, NCORES)
    res = run_bass_kernel_spmd(nc, maps, core_ids=list(range(NCORES)))
    return np.concatenate([r["out"] for r in res.results], axis=0).astype(np.float32)
```
